# Optimizing a Trainium2 kernel written in Bass

```python
import math
import jax, jax.numpy as jnp
from jax import lax
import numpy as np

D_MODEL = 2048
BATCH = 2
SEQ = 4096
DEPTH = 2

CHUNK = 64
N_MEM = 256
CONV_W = 4
N_EVEN = (DEPTH + 1) // 2
N_ODD = DEPTH // 2
ALPHA = (2 * DEPTH) ** 0.25
BETA = (8 * DEPTH) ** -0.25
LN_EPS = 1e-5
RMS_EPS = 1e-6
DT_MIN, DT_MAX = 1e-3, 1e-1

W_A = D_MODEL // 2
H_A = 8
BW_A = W_A // H_A
RG_C = 8.0
W_B = D_MODEL
HD_B = 64
H_B = W_B // HD_B
NG_B = 2
N_B = 128
CONV_B = W_B + 2 * NG_B * N_B
IN_AB = 2 * W_A + W_B + CONV_B + H_B
OUT_AB = W_A + W_B
W_C = D_MODEL // 2
GS_C = 16
G_C = W_C // GS_C
P_C = 64
H_D = 8
DK_D = D_MODEL // 16
DV_D = D_MODEL // 16
W_D = H_D * DV_D
QKV_D = 2 * H_D * DK_D + W_D
IN_CD = W_C + QKV_D + W_D + 2 * H_D
OUT_CD = W_C + W_D
H_X = 4
HD_X = D_MODEL // H_X
NG_E = 4
E_PER = 8
N_EXP = NG_E * E_PER
D_E = D_MODEL // 8
TOPK_IN = 2

kernel_name = "hybrid_rglru_ssd_s5_gdn_hmoe_deepnorm"


def layer_norm(x, g, b):
    xf = x.astype(jnp.float32)
    mu = jnp.mean(xf, -1, keepdims=True)
    var = jnp.mean(jnp.square(xf - mu), -1, keepdims=True)
    return ((xf - mu) * lax.rsqrt(var + LN_EPS) * g + b).astype(x.dtype)


def rms_norm(x, g):
    xf = x.astype(jnp.float32)
    return (xf * lax.rsqrt(jnp.mean(xf * xf, -1, keepdims=True) + RMS_EPS) * g).astype(x.dtype)


def l2norm(x):
    return x * lax.rsqrt(jnp.sum(x * x, -1, keepdims=True) + 1e-6)


def causal_conv(x, w, b=None):
    K, C = w.shape
    xp = jnp.pad(x, ((0, 0), (K - 1, 0), (0, 0)))
    y = lax.conv_general_dilated(xp, w[:, None, :].astype(x.dtype), (1,), 'VALID',
                                 dimension_numbers=('NWC', 'WIO', 'NWC'),
                                 feature_group_count=C)
    return y if b is None else y + b


def segsum(a):
    cs = jnp.cumsum(a, -1)
    T = a.shape[-1]
    mask = jnp.tril(jnp.ones((T, T), bool))
    return jnp.where(mask, cs[..., :, None] - cs[..., None, :], -jnp.inf)


def rg_lru(x, wa, ba, wx, bx, lam):
    Bsz, S, _ = x.shape
    f32 = jnp.float32
    xh = x.reshape(Bsz, S, H_A, BW_A)
    r = jax.nn.sigmoid(jnp.einsum('bshi,hij->bshj', xh, wa).reshape(Bsz, S, W_A) + ba)
    i = jax.nn.sigmoid(jnp.einsum('bshi,hij->bshj', xh, wx).reshape(Bsz, S, W_A) + bx)
    log_a = -RG_C * r.astype(f32) * jax.nn.softplus(-lam.astype(f32))
    a = jnp.exp(log_a)
    u = jnp.sqrt(-jnp.expm1(2.0 * log_a)) * (i * x).astype(f32)

    def comb(e1, e2):
        a1, b1 = e1
        a2, b2 = e2
        return a1 * a2, a2 * b1 + b2

    _, h = lax.associative_scan(comb, (a, u), axis=1)
    return h.astype(x.dtype)


def ssd_mixer(xbc, z, dt_raw, conv_w, conv_b, dt_bias, a_log, d, norm_w):
    Bsz, S, _ = xbc.shape
    f32 = jnp.float32
    NC = S // CHUNK
    HG = H_B // NG_B
    xbc = jax.nn.silu(causal_conv(xbc, conv_w, conv_b))
    xs, Bm, Cm = jnp.split(xbc, [W_B, W_B + NG_B * N_B], axis=-1)
    dt = jax.nn.softplus((dt_raw + dt_bias).astype(f32))
    A = -jnp.exp(a_log.astype(f32))
    X = xs.reshape(Bsz, NC, CHUNK, NG_B, HG, HD_B).astype(f32)
    Bc = Bm.reshape(Bsz, NC, CHUNK, NG_B, N_B).astype(f32)
    Cc = Cm.reshape(Bsz, NC, CHUNK, NG_B, N_B).astype(f32)
    dtc = dt.reshape(Bsz, NC, CHUNK, NG_B, HG)
    Xdt = X * dtc[..., None]
    Adt = jnp.moveaxis(dtc * A.reshape(NG_B, HG), 2, -1)
    A_cs = jnp.cumsum(Adt, -1)
    Lmat = jnp.exp(segsum(Adt))
    CB = jnp.einsum('bclgn,bcsgn->bcgls', Cc, Bc)
    y_diag = jnp.einsum('bcgls,bcgjls,bcsgjp->bclgjp', CB, Lmat, Xdt)
    decay_states = jnp.exp(A_cs[..., -1:] - A_cs)
    chunk_states = jnp.einsum('bclgn,bcgjl,bclgjp->bcgjpn', Bc, decay_states, Xdt)
    chunk_decay = jnp.exp(A_cs[..., -1])

    def step(state, inp):
        cst, dec = inp
        return state * dec[..., None, None] + cst, state

    init = jnp.zeros((Bsz, NG_B, HG, HD_B, N_B), f32)
    _, prev = lax.scan(step, init, (jnp.moveaxis(chunk_states, 1, 0), jnp.moveaxis(chunk_decay, 1, 0)))
    prev = jnp.moveaxis(prev, 0, 1)
    y_off = jnp.einsum('bclgn,bcgjpn,bcgjl->bclgjp', Cc, prev, jnp.exp(A_cs))
    y = (y_diag + y_off).reshape(Bsz, S, W_B) + xs.astype(f32) * jnp.repeat(d.astype(f32), HD_B)
    yg = (y * jax.nn.silu(z.astype(f32))).reshape(Bsz, S, NG_B, W_B // NG_B)
    y = rms_norm(yg, norm_w.reshape(NG_B, W_B // NG_B)).reshape(Bsz, S, W_B)
    return y.astype(xbc.dtype)


def s5_mixer(u, a_re, a_im, log_step, b_re, b_im, c_re, c_im, d, glu_w, glu_b):
    Bsz, S, _ = u.shape
    f32 = jnp.float32
    ar, ai = a_re.astype(f32), a_im.astype(f32)
    step = jnp.exp(log_step.astype(f32))[:, None]
    mag = jnp.exp(ar * step)
    lb_re, lb_im = mag * jnp.cos(ai * step), mag * jnp.sin(ai * step)
    den = ar * ar + ai * ai
    f_re = ((lb_re - 1.0) * ar + lb_im * ai) / den
    f_im = (lb_im * ar - (lb_re - 1.0) * ai) / den
    br, bi = b_re.astype(f32), b_im.astype(f32)
    bb_re = f_re[..., None] * br - f_im[..., None] * bi
    bb_im = f_re[..., None] * bi + f_im[..., None] * br
    uh = u.reshape(Bsz, S, G_C, GS_C).astype(f32)
    bu_re = jnp.einsum('bsgk,gpk->bsgp', uh, bb_re)
    bu_im = jnp.einsum('bsgk,gpk->bsgp', uh, bb_im)
    la_re = jnp.broadcast_to(lb_re, bu_re.shape)
    la_im = jnp.broadcast_to(lb_im, bu_im.shape)

    def comb(e1, e2):
        a1r, a1i, b1r, b1i = e1
        a2r, a2i, b2r, b2i = e2
        return (a2r * a1r - a2i * a1i, a2r * a1i + a2i * a1r,
                a2r * b1r - a2i * b1i + b2r, a2r * b1i + a2i * b1r + b2i)

    _, _, h_re, h_im = lax.associative_scan(comb, (la_re, la_im, bu_re, bu_im), axis=1)
    y = (jnp.einsum('gkp,bsgp->bsgk', c_re.astype(f32), h_re)
         - jnp.einsum('gkp,bsgp->bsgk', c_im.astype(f32), h_im)).reshape(Bsz, S, W_C)
    y = (y + d * u.astype(f32)).astype(u.dtype)
    g = jax.nn.gelu(y)
    return g * jax.nn.sigmoid(g @ glu_w + glu_b)


def gated_deltanet(qkv, z, a_raw, b_raw, conv_w, a_log, dt_bias, norm_w):
    Bsz, S, _ = qkv.shape
    f32 = jnp.float32
    NC = S // CHUNK
    qkv = jax.nn.silu(causal_conv(qkv, conv_w))
    q, k, v = jnp.split(qkv, [H_D * DK_D, 2 * H_D * DK_D], -1)
    q = l2norm(q.reshape(Bsz, S, H_D, DK_D).astype(f32)) * (DK_D ** -0.5)
    k = l2norm(k.reshape(Bsz, S, H_D, DK_D).astype(f32))
    v = v.reshape(Bsz, S, H_D, DV_D).astype(f32)
    g = -jnp.exp(a_log.astype(f32)) * jax.nn.softplus((a_raw + dt_bias).astype(f32))
    beta = jax.nn.sigmoid(b_raw.astype(f32))

    def chunks(t):
        return jnp.moveaxis(t.reshape(Bsz, NC, CHUNK, H_D, -1), 3, 1)

    qc, kc, vc = chunks(q), chunks(k), chunks(v)
    gcs = jnp.cumsum(chunks(g[..., None])[..., 0], -1)
    bc = chunks(beta[..., None])[..., 0]
    tri_incl = jnp.tril(jnp.ones((CHUNK, CHUNK), bool))
    tri_strict = jnp.tril(jnp.ones((CHUNK, CHUNK), bool), -1)
    decay = jnp.exp(jnp.where(tri_incl, gcs[..., :, None] - gcs[..., None, :], -jnp.inf))
    kb = kc * bc[..., None]
    kk = jnp.einsum('bhnld,bhnsd->bhnls', kb, kc) * decay
    m = jnp.where(tri_strict, kk, 0.0) + jnp.eye(CHUNK, dtype=f32)
    rhs = jnp.concatenate([vc * bc[..., None], kb * jnp.exp(gcs)[..., None]], -1)
    sol = lax.linalg.triangular_solve(m, rhs, left_side=True, lower=True, unit_diagonal=True)
    u_c, w_c = sol[..., :DV_D], sol[..., DV_D:]
    qk = jnp.where(tri_incl, jnp.einsum('bhnld,bhnsd->bhnls', qc, kc) * decay, 0.0)
    q_dec = qc * jnp.exp(gcs)[..., None]
    k_dec = kc * jnp.exp(gcs[..., -1:] - gcs)[..., None]
    g_last = jnp.exp(gcs[..., -1])

    def step(state, inp):
        w_i, u_i, qk_i, qd_i, kd_i, gl_i = inp
        v_new = u_i - jnp.einsum('bhld,bhdv->bhlv', w_i, state)
        o = jnp.einsum('bhld,bhdv->bhlv', qd_i, state) + jnp.einsum('bhls,bhsv->bhlv', qk_i, v_new)
        state = state * gl_i[..., None, None] + jnp.einsum('bhld,bhlv->bhdv', kd_i, v_new)
        return state, o

    xs = tuple(jnp.moveaxis(t, 2, 0) for t in (w_c, u_c, qk, q_dec, k_dec, g_last))
    _, o = lax.scan(step, jnp.zeros((Bsz, H_D, DK_D, DV_D), f32), xs)
    o = jnp.moveaxis(jnp.moveaxis(o, 0, 2), 1, 3).reshape(Bsz, S, H_D, DV_D)
    o = rms_norm(o, norm_w) * jax.nn.silu(z.reshape(Bsz, S, H_D, DV_D).astype(f32))
    return o.reshape(Bsz, S, W_D).astype(qkv.dtype)


def mixer_ab(x, w_in, rg_conv_w, rg_conv_b, rg_wa, rg_ba, rg_wx, rg_bx, rg_lam,
             ssd_conv_w, ssd_conv_b, ssd_dt_bias, ssd_a_log, ssd_d, ssd_norm_w, w_out):
    proj = x @ w_in
    gate_a, xa, z_b, xbc_b, dt_b = jnp.split(
        proj, [W_A, 2 * W_A, 2 * W_A + W_B, 2 * W_A + W_B + CONV_B], -1)
    ya = jax.nn.gelu(gate_a) * rg_lru(causal_conv(xa, rg_conv_w, rg_conv_b),
                                      rg_wa, rg_ba, rg_wx, rg_bx, rg_lam)
    yb = ssd_mixer(xbc_b, z_b, dt_b, ssd_conv_w, ssd_conv_b, ssd_dt_bias, ssd_a_log, ssd_d, ssd_norm_w)
    return jnp.concatenate([ya, yb], -1) @ w_out


def mixer_cd(x, w_in, s5_a_re, s5_a_im, s5_log_step, s5_b_re, s5_b_im, s5_c_re, s5_c_im, s5_d,
             s5_glu_w, s5_glu_b, dn_conv_w, dn_a_log, dn_dt_bias, dn_norm_w, w_out):
    proj = x @ w_in
    u_c, qkv, z_d, a_d, b_d = jnp.split(
        proj, [W_C, W_C + QKV_D, W_C + QKV_D + W_D, W_C + QKV_D + W_D + H_D], -1)
    yc = s5_mixer(u_c, s5_a_re, s5_a_im, s5_log_step, s5_b_re, s5_b_im, s5_c_re, s5_c_im,
                  s5_d, s5_glu_w, s5_glu_b)
    yd = gated_deltanet(qkv, z_d, a_d, b_d, dn_conv_w, dn_a_log, dn_dt_bias, dn_norm_w)
    return jnp.concatenate([yc, yd], -1) @ w_out


def mem_xattn(x, mem, w_q, w_kv, w_o):
    Bsz, S, _ = x.shape
    M = mem.shape[1]
    q = (x @ w_q).reshape(Bsz, S, H_X, HD_X)
    k, v = jnp.split(mem @ w_kv, 2, -1)
    k = k.reshape(Bsz, M, H_X, HD_X)
    v = v.reshape(Bsz, M, H_X, HD_X)
    s = jnp.einsum('bshd,bmhd->bhsm', q, k).astype(jnp.float32) * (HD_X ** -0.5)
    p = jax.nn.softmax(s, -1).astype(x.dtype)
    o = jnp.einsum('bhsm,bmhd->bshd', p, v).reshape(Bsz, S, D_MODEL)
    return o @ w_o


def hier_moe(x, w_group, b_group, w_expert, b_expert, w_gate, w_up, w_down):
    Bsz, S, D = x.shape
    f32 = jnp.float32
    t = x.reshape(-1, D)
    gp = jax.nn.softmax((t @ w_group + b_group).astype(f32), -1)
    g_idx = jnp.argmax(gp, -1)
    g_prob = jnp.take_along_axis(gp, g_idx[:, None], -1)
    el = (t @ w_expert + b_expert).astype(f32).reshape(-1, NG_E, E_PER)
    el_sel = jnp.take_along_axis(el, g_idx[:, None, None], 1)[:, 0]
    top_v, top_i = lax.top_k(el_sel, TOPK_IN)
    top_w = jax.nn.softmax(top_v, -1) * g_prob
    within = jnp.sum(jax.nn.one_hot(top_i, E_PER, dtype=f32) * top_w[..., None], 1)
    combine = (jax.nn.one_hot(g_idx, NG_E, dtype=f32)[:, :, None] * within[:, None, :]).reshape(-1, N_EXP)
    h = jax.nn.silu(jnp.einsum('td,edf->tef', t, w_gate)) * jnp.einsum('td,edf->tef', t, w_up)
    y = jnp.einsum('tef,te,efd->td', h, combine.astype(x.dtype), w_down)
    return y.reshape(Bsz, S, D)


def setup_inputs(seed: int = 0) -> dict:
    key = jax.random.key(seed)
    ks = iter(jax.random.split(key, 64))
    f32 = jnp.float32

    def nrm(shape, scale=1.0):
        return jax.random.normal(next(ks), shape, f32) * scale

    def unif(shape, lo, hi):
        return jax.random.uniform(next(ks), shape, f32, lo, hi)

    def dt_bias_init(shape):
        dt = jnp.exp(unif(shape, math.log(DT_MIN), math.log(DT_MAX)))
        return dt + jnp.log(-jnp.expm1(-dt))

    NE, NO, L, D = N_EVEN, N_ODD, DEPTH, D_MODEL
    a_rg = unif((NE, W_A), 0.9, 0.999) ** (1.0 / RG_C)
    xa_k = nrm((L, D, D), D ** -0.5)
    xa_v = nrm((L, D, D), D ** -0.5 * BETA)
    return {
        "x": nrm((BATCH, SEQ, D)),
        "mem": nrm((BATCH, N_MEM, D)),
        "ab_w_in": nrm((NE, D, IN_AB), D ** -0.5),
        "rg_conv_w": nrm((NE, CONV_W, W_A), CONV_W ** -0.5),
        "rg_conv_b": nrm((NE, W_A), 0.01),
        "rg_wa": nrm((NE, H_A, BW_A, BW_A), BW_A ** -0.5),
        "rg_ba": nrm((NE, W_A), 0.01),
        "rg_wx": nrm((NE, H_A, BW_A, BW_A), BW_A ** -0.5),
        "rg_bx": nrm((NE, W_A), 0.01),
        "rg_lam": jnp.log(a_rg) - jnp.log1p(-a_rg),
        "ssd_conv_w": nrm((NE, CONV_W, CONV_B), CONV_W ** -0.5),
        "ssd_conv_b": nrm((NE, CONV_B), 0.01),
        "ssd_dt_bias": dt_bias_init((NE, H_B)),
        "ssd_a_log": jnp.log(unif((NE, H_B), 1.0, 16.0)),
        "ssd_d": 1.0 + nrm((NE, H_B), 0.01),
        "ssd_norm_w": 1.0 + nrm((NE, W_B), 0.01),
        "ab_w_out": nrm((NE, OUT_AB, D), OUT_AB ** -0.5 * BETA),
        "cd_w_in": nrm((NO, D, IN_CD), D ** -0.5),
        "s5_a_re": -0.5 + nrm((NO, G_C, P_C), 0.01),
        "s5_a_im": jnp.pi * jnp.arange(P_C, dtype=f32) + nrm((NO, G_C, P_C), 0.01),
        "s5_log_step": unif((NO, G_C), math.log(DT_MIN), math.log(DT_MAX)),
        "s5_b_re": nrm((NO, G_C, P_C, GS_C), (2 * GS_C) ** -0.5),
        "s5_b_im": nrm((NO, G_C, P_C, GS_C), (2 * GS_C) ** -0.5),
        "s5_c_re": nrm((NO, G_C, GS_C, P_C), P_C ** -0.5),
        "s5_c_im": nrm((NO, G_C, GS_C, P_C), P_C ** -0.5),
        "s5_d": nrm((NO, W_C)),
        "s5_glu_w": nrm((NO, W_C, W_C), W_C ** -0.5),
        "s5_glu_b": nrm((NO, W_C), 0.01),
        "dn_conv_w": nrm((NO, CONV_W, QKV_D), CONV_W ** -0.5),
        "dn_a_log": jnp.log(unif((NO, H_D), 1.0, 16.0)),
        "dn_dt_bias": dt_bias_init((NO, H_D)),
        "dn_norm_w": 1.0 + nrm((NO, DV_D), 0.01),
        "cd_w_out": nrm((NO, OUT_CD, D), OUT_CD ** -0.5 * BETA),
        "xa_w_q": nrm((L, D, D), D ** -0.5),
        "xa_w_kv": jnp.concatenate([xa_k, xa_v], -1),
        "xa_w_o": nrm((L, D, D), D ** -0.5 * BETA),
        "moe_w_group": nrm((L, D, NG_E), D ** -0.5),
        "moe_b_group": nrm((L, NG_E), 0.01),
        "moe_w_expert": nrm((L, D, N_EXP), D ** -0.5),
        "moe_b_expert": nrm((L, N_EXP), 0.01),
        "moe_w_gate": nrm((L, N_EXP, D, D_E), D ** -0.5),
        "moe_w_up": nrm((L, N_EXP, D, D_E), D ** -0.5),
        "moe_w_down": nrm((L, N_EXP, D_E, D), D_E ** -0.5 * BETA),
        "ln1_g": 1.0 + nrm((L, D), 0.01),
        "ln1_b": nrm((L, D), 0.01),
        "ln2_g": 1.0 + nrm((L, D), 0.01),
        "ln2_b": nrm((L, D), 0.01),
        "ln3_g": 1.0 + nrm((L, D), 0.01),
        "ln3_b": nrm((L, D), 0.01),
    }


def reference(x, mem,
              ab_w_in, rg_conv_w, rg_conv_b, rg_wa, rg_ba, rg_wx, rg_bx, rg_lam,
              ssd_conv_w, ssd_conv_b, ssd_dt_bias, ssd_a_log, ssd_d, ssd_norm_w, ab_w_out,
              cd_w_in, s5_a_re, s5_a_im, s5_log_step, s5_b_re, s5_b_im, s5_c_re, s5_c_im, s5_d,
              s5_glu_w, s5_glu_b, dn_conv_w, dn_a_log, dn_dt_bias, dn_norm_w, cd_w_out,
              xa_w_q, xa_w_kv, xa_w_o,
              moe_w_group, moe_b_group, moe_w_expert, moe_b_expert, moe_w_gate, moe_w_up, moe_w_down,
              ln1_g, ln1_b, ln2_g, ln2_b, ln3_g, ln3_b):
    for l in range(DEPTH):
        i = l // 2
        if l % 2 == 0:
            mix = mixer_ab(x, ab_w_in[i], rg_conv_w[i], rg_conv_b[i], rg_wa[i], rg_ba[i], rg_wx[i],
                           rg_bx[i], rg_lam[i], ssd_conv_w[i], ssd_conv_b[i], ssd_dt_bias[i],
                           ssd_a_log[i], ssd_d[i], ssd_norm_w[i], ab_w_out[i])
        else:
            mix = mixer_cd(x, cd_w_in[i], s5_a_re[i], s5_a_im[i], s5_log_step[i], s5_b_re[i],
                           s5_b_im[i], s5_c_re[i], s5_c_im[i], s5_d[i], s5_glu_w[i], s5_glu_b[i],
                           dn_conv_w[i], dn_a_log[i], dn_dt_bias[i], dn_norm_w[i], cd_w_out[i])
        x = layer_norm(ALPHA * x + mix, ln1_g[l], ln1_b[l])
        x = layer_norm(ALPHA * x + mem_xattn(x, mem, xa_w_q[l], xa_w_kv[l], xa_w_o[l]), ln2_g[l], ln2_b[l])
        x = layer_norm(ALPHA * x + hier_moe(x, moe_w_group[l], moe_b_group[l], moe_w_expert[l],
                                            moe_b_expert[l], moe_w_gate[l], moe_w_up[l], moe_w_down[l]),
                       ln3_g[l], ln3_b[l])
    return x
```

```python
import contextlib
import numpy as np
import concourse.bass as bass
import concourse.mybir as mybir

F32 = mybir.dt.float32
F32R = mybir.dt.float32r
AF = mybir.ActivationFunctionType
ALU = mybir.AluOpType
AX = mybir.AxisListType

COMPUTE = ("pe", "act", "dve", "pool")
DMAQ = ("sp", "qact", "qpool")
ALPHA = 4.0 ** 0.25
LN_EPS = 1e-5
RMS_EPS = 1e-6


class Prog:
    def __init__(self, nc, ring=8, same_engine_sync=True):
        self.nc = nc
        self.ops = []
        self.cnt = {e: 0 for e in COMPUTE}
        self.ring = ring
        self.dma_i = {q: 0 for q in DMAQ}
        self.dma_val = {}
        self.last_w = {}
        self.readers = {}
        self.waited = {}
        self.same = same_engine_sync
        self.semkeys = set()

    @staticmethod
    def stream_of(eng):
        return {"pe": "pe", "act": "act", "dve": "dve", "pool": "pool",
                "sp": "sp", "qact": "act", "qpool": "pool"}[eng]

    def _need(self, stream, dep, waits):
        if dep is None:
            return
        semkey, val, dep_stream, is_dma = dep
        if (not is_dma) and dep_stream == stream and not self.same:
            return
        k = (stream, semkey)
        if self.waited.get(k, 0) >= val:
            return
        self.waited[k] = val
        waits.append((semkey, val))

    def op(self, eng, fn, reads=(), writes=()):
        stream = self.stream_of(eng)
        waits = []
        for r in reads:
            self._need(stream, self.last_w.get(r), waits)
        for w in writes:
            self._need(stream, self.last_w.get(w), waits)
            for d in self.readers.get(w, ()):
                self._need(stream, d, waits)
        if eng in COMPUTE:
            self.cnt[eng] += 1
            semkey = ("c", eng)
            val = self.cnt[eng]
            inc = (semkey, 1)
            dep = (semkey, val, stream, False)
        else:
            i = self.dma_i[eng]
            self.dma_i[eng] += 1
            k = i % self.ring
            semkey = ("d", eng, k)
            prev = self.dma_val.get(semkey, 0)
            if prev > 0:
                self._need(stream, (semkey, prev, stream, True), waits)
            val = prev + 16
            self.dma_val[semkey] = val
            inc = (semkey, 16)
            dep = (semkey, val, stream, True)
        self.semkeys.add(semkey)
        for r in reads:
            self.readers.setdefault(r, []).append(dep)
        for w in writes:
            self.last_w[w] = dep
            self.readers[w] = []
        self.ops.append((eng, stream, fn, waits, inc))
        return dep

    def final_waits(self, stream="sp"):
        waits = []
        for semkey, val in list(self.dma_val.items()):
            self._need(stream, (semkey, val, None, True), waits)
        for e in COMPUTE:
            if self.cnt[e]:
                self._need(stream, (("c", e), self.cnt[e], None, True), waits)
        self.ops.append((stream, stream, None, waits, None))

    def emit(self):
        nc = self.nc
        with contextlib.ExitStack() as es:
            sems = {}
            for sk in sorted(self.semkeys, key=str):
                sems[sk] = es.enter_context(nc.semaphore("s_" + "_".join(map(str, sk))))
            block = es.enter_context(nc.Block())
            streams = {"pe": [], "act": [], "dve": [], "pool": [], "sp": []}
            for o in self.ops:
                streams[o[1]].append(o)

            def run(engobj, lst):
                for (eng, stream, fn, waits, inc) in lst:
                    for (sk, val) in waits:
                        engobj.wait_ge(sems[sk], val)
                    if fn is not None:
                        ins = fn(engobj)
                        ins.then_inc(sems[inc[0]], inc[1])

            @block.tensor
            def _(e):
                run(e, streams["pe"])

            @block.scalar
            def _(e):
                run(e, streams["act"])

            @block.vector
            def _(e):
                run(e, streams["dve"])

            @block.gpsimd
            def _(e):
                run(e, streams["pool"])

            @block.sync
            def _(e):
                run(e, streams["sp"])


class K:
    def __init__(self, P):
        self.P = P

    def dma(self, q, out, in_, reads=(), writes=()):
        self.P.op(q, lambda e: e.dma_start(out=out, in_=in_), reads, writes)

    def mm(self, out, lhsT, rhs, start, stop, reads=(), writes=()):
        self.P.op("pe", lambda e: e.matmul(out, lhsT, rhs, start=start, stop=stop), reads, writes)

    def tr(self, out, in_, ident, reads=(), writes=()):
        self.P.op("pe", lambda e: e.transpose(out, in_, ident), reads, writes)

    def act(self, out, in_, func, reads=(), writes=(), bias=None, scale=None, accum_out=None):
        kw = {}
        if bias is not None:
            kw["bias"] = bias
        if scale is not None:
            kw["scale"] = scale
        if accum_out is not None:
            kw["accum_out"] = accum_out
        self.P.op("act", lambda e: e.activation(out=out, in_=in_, func=func, **kw), reads, writes)

    def tt(self, eng, out, in0, in1, op, reads=(), writes=()):
        self.P.op(eng, lambda e: e.tensor_tensor(out=out, in0=in0, in1=in1, op=op), reads, writes)

    def ts(self, eng, out, in0, s1, s2, op0, op1=None, reads=(), writes=()):
        if op1 is None:
            self.P.op(eng, lambda e: e.tensor_scalar(out=out, in0=in0, scalar1=s1, scalar2=None, op0=op0), reads, writes)
        else:
            self.P.op(eng, lambda e: e.tensor_scalar(out=out, in0=in0, scalar1=s1, scalar2=s2, op0=op0, op1=op1), reads, writes)

    def stt(self, eng, out, in0, scalar, in1, op0, op1, reads=(), writes=()):
        self.P.op(eng, lambda e: e.scalar_tensor_tensor(out=out, in0=in0, scalar=scalar, in1=in1, op0=op0, op1=op1), reads, writes)

    def copy(self, eng, out, in_, reads=(), writes=()):
        if eng == "act":
            self.P.op("act", lambda e: e.activation(out=out, in_=in_, func=AF.Copy), reads, writes)
        else:
            self.P.op(eng, lambda e: e.tensor_copy(out=out, in_=in_), reads, writes)

    def memset(self, eng, out, val, writes=()):
        self.P.op(eng, lambda e: e.memset(out, val), (), writes)

    def scan(self, out, d0, d1, init, op0, op1, reads=(), writes=()):
        self.P.op("dve", lambda e: e.tensor_tensor_scan(out, d0, d1, init, op0, op1), reads, writes)

    def reduce(self, eng, out, in_, op, axis=AX.X, reads=(), writes=()):
        self.P.op(eng, lambda e: e.tensor_reduce(out=out, in_=in_, axis=axis, op=op), reads, writes)


def R(ap):
    return ap.bitcast(F32R)


GK = 1.5957691216057308


def gelu_ops(k, out, x, tmp, rx, rout, rtmp, n=128):
    k.act(tmp, x, AF.Square, reads=[rx], writes=[rtmp])
    k.ts("dve", tmp, tmp, 0.044715, 1.0, ALU.mult, ALU.add, reads=[rtmp], writes=[rtmp])
    k.tt("dve", tmp, tmp, x, ALU.mult, reads=[rtmp, rx], writes=[rtmp])
    k.act(tmp, tmp, AF.Sigmoid, scale=GK, reads=[rtmp], writes=[rtmp])
    k.tt("pool", out, tmp, x, ALU.mult, reads=[rtmp, rx], writes=[rout])


def conv_ops(k, acc, ext, pq, c0, rext, racc, eng="dve"):
    k.ts(eng, acc, ext[:, 0:512], pq[:, c0:c0 + 1], None, ALU.mult, reads=[rext, "pq"], writes=[racc])
    for i in range(1, 4):
        k.stt(eng, acc, ext[:, i:i + 512], pq[:, c0 + i:c0 + i + 1], acc, ALU.mult, ALU.add,
              reads=[rext, "pq", racc], writes=[racc])


def build_p1ab(nbatch=2, nkb=8):
    nc = bass.Bass("TRN2", target_bir_lowering=False)

    def D(name, shape, kind="ExternalInput"):
        return nc.dram_tensor(name, shape, F32, kind=kind).ap()

    xT = D("xT", [2048, 8192])
    w1d = D("w1", [2048, 1028])
    rgwd = D("rgw", [128, 256])
    pqd = D("pq", [128, 48])
    cstd = D("cst", [128, 1408])
    ya_o = D("ya", [128, 8192], "ExternalOutput")
    yb_o = D("yb", [4, 64, 8192], "ExternalOutput")

    with contextlib.ExitStack() as es:
        def S(name, shape):
            return es.enter_context(nc.sbuf_tensor(name + "_s", shape, F32))

        w1 = S("w1", [128, 16 * 1028])
        xb = [S("xb0", [128, 16 * 512])] * 2
        rgw = S("rgw", [128, 256])
        pq = S("pq", [128, 48])
        cst = S("cst", [128, 1408])
        xa_ext = S("xa_ext", [128, 515])
        B_ext = S("B_ext", [128, 515])
        C_ext = S("C_ext", [128, 515])
        xs_ext = S("xs_ext", [64, 4 * 515])
        xg = S("xg", [128, 512])
        ga = S("ga", [128, 512])
        gt = S("gt", [128, 512])
        xc = S("xc", [128, 512])
        r_sb = S("r_sb", [128, 512])
        i_sb = S("i_sb", [128, 512])
        a_sb = S("a_sb", [128, 512])
        u_sb = S("u_sb", [128, 512])
        h_sb = S("h_sb", [128, 512])
        ya_sb = S("ya_sb", [128, 512])
        hprev = S("hprev", [128, 1])
        zs = S("zs", [64, 4 * 512])
        xs_fm = S("xs_fm", [64, 4 * 512])
        xsD = S("xsD", [64, 4 * 512])
        acc = S("acc", [128, 512])
        B_fm = S("B_fm", [128, 512])
        C_fm = S("C_fm", [128, 512])
        dt_fm = S("dt_fm", [4, 512])
        adt = S("adt", [4, 512])
        cs_fm = S("cs_fm", [4, 512])
        sm = S("smab", [128, 16])
        dtcs = S("dtcs", [64, 8])
        Xdt = S("Xdt", [64, 256])
        ecsb = S("ecsb", [128, 256])
        dk = S("dk", [64, 4])
        Bdec = S("Bdec", [64, 4 * 128])
        df = S("df", [64, 256])
        MT = S("MT", [64, 256])
        Cdec = S("Cdec", [128, 256])
        yv = S("yv", [64, 256])
        yb_blk = S("yb_blk", [64, 4 * 512])
        S_sb = S("S_sb", [128, 256])
        ps = [es.enter_context(nc.psum_tensor(f"ps{i}", [128, 512], F32)) for i in range(8)]

        ones = cst[:, 0:128]
        ident = cst[:, 128:256]
        mask = cst[0:64, 256:320]
        sel4 = cst[0:4, 320:832]
        rmask = cst[0:4, 832:1344]
        onecol = cst[:, 0:1]
        w13 = w1[:].rearrange("p (c n) -> p c n", n=1028)
        xs_ext3 = xs_ext[:].rearrange("p (h t) -> p h t", t=515)
        zs3 = zs[:].rearrange("p (h t) -> p h t", t=512)
        xs3 = xs_fm[:].rearrange("p (h t) -> p h t", t=512)
        xsD3 = xsD[:].rearrange("p (h t) -> p h t", t=512)
        yb3 = yb_blk[:].rearrange("p (h t) -> p h t", t=512)
        Xdt3 = Xdt[:].rearrange("p (h q) -> p h q", q=64)
        ecsb3 = ecsb[:].rearrange("p (h l) -> p h l", l=64)
        Bdec3 = Bdec[:].rearrange("p (h n) -> p h n", n=128)
        df3 = df[:].rearrange("p (h l) -> p h l", l=64)
        MT3 = MT[:].rearrange("p (h l) -> p h l", l=64)
        Cdec3 = Cdec[:].rearrange("p (h l) -> p h l", l=64)
        yv3 = yv[:].rearrange("p (h l) -> p h l", l=64)
        S3 = S_sb[:].rearrange("p (h q) -> p h q", q=64)

        P = Prog(nc)
        k = K(P)
        for q4 in range(4):
            k.dma("qpool", R(w13[:, 4 * q4:4 * q4 + 4, :]),
                  w1d[q4 * 512:(q4 + 1) * 512, :].rearrange("(c p) n -> p c n", p=128), writes=["w1"])
        k.dma("qpool", R(rgw[:]), rgwd[:, :], writes=["rgw"])
        k.dma("sp", pq[:], pqd[:, :], writes=["pq"])
        k.dma("sp", cst[:], cstd[:, :], writes=["cst"])
        m8sp = sm[:, 0:1]
        Aneg = sm[0:4, 1:2]
        k.act(m8sp, pq[:, 7:8], AF.Exp, scale=-1.0, reads=["pq"], writes=["sm0"])
        k.act(m8sp, m8sp, AF.Ln, bias=onecol, reads=["sm0", "cst"], writes=["sm0"])
        k.ts("dve", m8sp, m8sp, -8.0, None, ALU.mult, reads=["sm0"], writes=["sm0"])
        k.act(Aneg, pq[0:4, 43:44], AF.Exp, reads=["pq"], writes=["sm1"])
        k.ts("dve", Aneg, Aneg, -1.0, None, ALU.mult, reads=["sm1"], writes=["sm1"])

        for b in range(nbatch):
            k.memset("dve", xa_ext[:, 0:3], 0.0, writes=["xa_ext"])
            k.memset("dve", B_ext[:, 0:3], 0.0, writes=["B_ext"])
            k.memset("dve", C_ext[:, 0:3], 0.0, writes=["C_ext"])
            k.memset("dve", xs_ext[:], 0.0, writes=["xs_ext"])
            k.memset("dve", hprev[:], 0.0, writes=["hprev"])
            k.ts("dve", R(S_sb[:]), cst[:, 0:256], 0.0, None, ALU.mult, reads=["cst"], writes=["S"])
            for kb in range(nkb):
                tok0 = b * 4096 + kb * 512
                bi = (b * nkb + kb) % 2
                xbv = xb[bi][:].rearrange("p (c t) -> p c t", t=512)
                rxb = "xb0"
                for q4 in range(4):
                    k.dma("qpool", R(xbv[:, 4 * q4:4 * q4 + 4, :]),
                          xT[q4 * 512:(q4 + 1) * 512, tok0:tok0 + 512].rearrange("(c p) t -> p c t", p=128),
                          writes=[rxb])

                pcount = [0]

                def proj(col0, M):
                    pn = pcount[0] % 4
                    pcount[0] += 1
                    for kc in range(16):
                        k.mm(ps[pn][0:M, :], R(w13[:, kc, col0:col0 + M]), R(xbv[:, kc, :]),
                             kc == 0, kc == 15, reads=["w1", rxb], writes=[f"ps{pn}"])
                    return ps[pn], f"ps{pn}"

                pt, pr = proj(0, 128)
                k.copy("act", xg[:], pt[:], reads=[pr], writes=["xg"])
                gelu_ops(k, ga[:], xg[:], gt[:], "xg", "ga", "gt")
                pt, pr = proj(128, 128)
                k.copy("act", xa_ext[:, 3:515], pt[:], reads=[pr], writes=["xa_ext"])
                for h in range(4):
                    pt, pr = proj(256 + 64 * h, 64)
                    k.act(zs3[:, h, :], pt[0:64, :], AF.Silu, reads=[pr], writes=["zs"])
                for h in range(4):
                    pt, pr = proj(512 + 64 * h, 64)
                    k.copy("act", xs_ext3[:, h, 3:515], pt[0:64, :], reads=[pr], writes=["xs_ext"])
                pt, pr = proj(768, 128)
                k.copy("act", B_ext[:, 3:515], pt[:], reads=[pr], writes=["B_ext"])
                pt, pr = proj(896, 128)
                k.copy("dve", C_ext[:, 3:515], pt[:], reads=[pr], writes=["C_ext"])
                pt, pr = proj(1024, 4)
                k.act(dt_fm[:], pt[0:4, :], AF.Exp, bias=pq[0:4, 42:43], reads=[pr, "pq"], writes=["dt_fm"])
                k.act(dt_fm[:], dt_fm[:], AF.Ln, bias=onecol[0:4, :], reads=["dt_fm", "cst"], writes=["dt_fm"])
                k.ts("dve", adt[:], dt_fm[:], Aneg, None, ALU.mult, reads=["dt_fm", "sm1"], writes=["adt"])
                k.scan(cs_fm[:], rmask, adt[:], 0.0, ALU.mult, ALU.add, reads=["adt", "cst"], writes=["cs_fm"])

                conv_ops(k, acc[:], xa_ext[:], pq, 0, "xa_ext", "acc")
                k.act(R(xc[:]), acc[:], AF.Identity, bias=pq[:, 4:5], reads=["acc", "pq"], writes=["xc"])
                k.copy("pool", xa_ext[:, 0:3], xa_ext[:, 512:515], reads=["xa_ext"], writes=["xa_ext"])
                k.mm(ps[4][:], R(rgw[:, 0:128]), R(xc[:]), True, True, reads=["rgw", "xc"], writes=["ps4"])
                k.act(r_sb[:], ps[4][:], AF.Sigmoid, bias=pq[:, 5:6], reads=["ps4", "pq"], writes=["r_sb"])
                k.mm(ps[5][:], R(rgw[:, 128:256]), R(xc[:]), True, True, reads=["rgw", "xc"], writes=["ps5"])
                k.act(i_sb[:], ps[5][:], AF.Sigmoid, bias=pq[:, 6:7], reads=["ps5", "pq"], writes=["i_sb"])
                k.act(a_sb[:], r_sb[:], AF.Exp, scale=m8sp, reads=["r_sb", "sm0"], writes=["a_sb"])
                k.tt("pool", r_sb[:], a_sb[:], a_sb[:], ALU.mult, reads=["a_sb"], writes=["r_sb"])
                k.ts("pool", r_sb[:], r_sb[:], 1.0, None, ALU.min, reads=["r_sb"], writes=["r_sb"])
                k.act(r_sb[:], r_sb[:], AF.Sqrt, scale=-1.0, bias=onecol, reads=["r_sb", "cst"], writes=["r_sb"])
                k.tt("dve", u_sb[:], i_sb[:], xc[:], ALU.mult, reads=["i_sb", "xc"], writes=["u_sb"])
                k.tt("dve", u_sb[:], u_sb[:], r_sb[:], ALU.mult, reads=["u_sb", "r_sb"], writes=["u_sb"])
                k.scan(h_sb[:], a_sb[:], u_sb[:], hprev[:, 0:1], ALU.mult, ALU.add,
                       reads=["a_sb", "u_sb", "hprev"], writes=["h_sb"])
                k.copy("pool", hprev[:], h_sb[:, 511:512], reads=["h_sb"], writes=["hprev"])
                k.tt("dve", ya_sb[:], ga[:], h_sb[:], ALU.mult, reads=["ga", "h_sb"], writes=["ya_sb"])
                k.dma("sp", ya_o[:, tok0:tok0 + 512], ya_sb[:], reads=["ya_sb"], writes=[("yao", tok0)])

                for h in range(4):
                    conv_ops(k, acc[0:64, :], xs_ext3[:, h, :], pq[0:64, :], 8 + 5 * h, "xs_ext", "acc",
                             eng="dve")
                    k.act(xs3[:, h, :], acc[0:64, :], AF.Silu, bias=pq[0:64, 12 + 5 * h:13 + 5 * h],
                          reads=["acc", "pq"], writes=["xs_fm"])
                    k.ts("pool", xsD3[:, h, :], xs3[:, h, :], pq[0:64, 38 + h:39 + h], None, ALU.mult,
                         reads=["xs_fm", "pq"], writes=["xsD"])
                k.copy("pool", xs_ext3[:, :, 0:3], xs_ext3[:, :, 512:515], reads=["xs_ext"], writes=["xs_ext"])
                conv_ops(k, acc[:], B_ext[:], pq, 28, "B_ext", "acc")
                k.act(R(B_fm[:]), acc[:], AF.Silu, bias=pq[:, 32:33], reads=["acc", "pq"], writes=["B_fm"])
                k.copy("pool", B_ext[:, 0:3], B_ext[:, 512:515], reads=["B_ext"], writes=["B_ext"])
                conv_ops(k, acc[:], C_ext[:], pq, 33, "C_ext", "acc")
                k.act(R(C_fm[:]), acc[:], AF.Silu, bias=pq[:, 37:38], reads=["acc", "pq"], writes=["C_fm"])
                k.copy("pool", C_ext[:, 0:3], C_ext[:, 512:515], reads=["C_ext"], writes=["C_ext"])

                for ch in range(8):
                    l0 = ch * 64
                    ls = slice(l0, l0 + 64)
                    k.tr(ps[4][0:64, 256:260], dt_fm[0:4, ls], ident[0:4, 0:4], reads=["dt_fm", "cst"], writes=["ps4"])
                    k.tr(ps[4][0:64, 260:264], cs_fm[0:4, ls], ident[0:4, 0:4], reads=["cs_fm", "cst"], writes=["ps4"])
                    k.copy("act", dtcs[:], ps[4][0:64, 256:264], reads=["ps4"], writes=["dtcs"])
                    for h in range(4):
                        k.tr(ps[4][0:64, h * 64:(h + 1) * 64], xs3[:, h, ls], ident[0:64, 0:64],
                             reads=["xs_fm", "cst"], writes=["ps4"])
                    k.tt("dve", R(Xdt3), ps[4][0:64, 0:256].rearrange("p (h q) -> p h q", q=64),
                         dtcs[:, 0:4].unsqueeze(2).to_broadcast([64, 4, 64]), ALU.mult,
                         reads=["ps4", "dtcs"], writes=["Xdt"])
                    for h in range(4):
                        k.mm(ps[5][:, h * 64:(h + 1) * 64], sel4[:, h * 128:(h + 1) * 128], cs_fm[0:4, ls],
                             True, True, reads=["cst", "cs_fm"], writes=["ps5"])
                    psc3 = ps[5][:, 0:256].rearrange("p (h l) -> p h l", l=64)
                    k.act(ecsb[:], ps[5][:, 0:256], AF.Exp, reads=["ps5"], writes=["ecsb"])
                    k.tt("dve", dk[:], psc3[0:64, :, 63], dtcs[:, 4:8], ALU.subtract,
                         reads=["ps5", "dtcs"], writes=["dk"])
                    k.act(dk[:], dk[:], AF.Exp, reads=["dk"], writes=["dk"])
                    k.tr(ps[6][0:64, 0:128], B_fm[:, ls], ident, reads=["B_fm", "cst"], writes=["ps6"])
                    for h in range(4):
                        if h % 2 == 0:
                            k.ts("dve", R(Bdec3[:, h, :]), ps[6][0:64, 0:128], dk[:, h:h + 1], None, ALU.mult,
                                 reads=["ps6", "dk"], writes=["Bdec"])
                        else:
                            k.act(R(Bdec3[:, h, :]), ps[6][0:64, 0:128], AF.Copy, scale=dk[:, h:h + 1],
                                  reads=["ps6", "dk"], writes=["Bdec"])
                    k.mm(ps[5][0:64, 256:320], R(B_fm[:, ls]), R(C_fm[:, ls]), True, True,
                         reads=["B_fm", "C_fm"], writes=["ps5"])
                    k.tt("dve", df3, psc3[0:64], dtcs[:, 4:8].unsqueeze(2).to_broadcast([64, 4, 64]), ALU.subtract,
                         reads=["ps5", "dtcs"], writes=["df"])
                    k.ts("dve", df[:], df[:], 0.0, None, ALU.min, reads=["df"], writes=["df"])
                    k.act(df[:], df[:], AF.Exp, reads=["df"], writes=["df"])
                    k.tt("pool", df3, df3, mask.unsqueeze(1).to_broadcast([64, 4, 64]), ALU.mult,
                         reads=["df", "cst"], writes=["df"])
                    k.tt("dve", R(MT3), df3, ps[5][0:64, 256:320].unsqueeze(1).to_broadcast([64, 4, 64]), ALU.mult,
                         reads=["df", "ps5"], writes=["MT"])
                    k.tt("pool", R(Cdec3), ecsb3, C_fm[:, ls].unsqueeze(1).to_broadcast([128, 4, 64]), ALU.mult,
                         reads=["ecsb", "C_fm"], writes=["Cdec"])
                    for h in range(4):
                        k.mm(ps[6][0:64, 128 + h * 64:128 + (h + 1) * 64], R(Xdt3[:, h, :]), R(MT3[:, h, :]),
                             True, False, reads=["Xdt", "MT"], writes=["ps6"])
                        k.mm(ps[6][0:64, 128 + h * 64:128 + (h + 1) * 64], R(S3[:, h, :]), R(Cdec3[:, h, :]),
                             False, True, reads=["S", "Cdec"], writes=["ps6"])
                    k.tt("dve", yv3, ps[6][0:64, 128:384].rearrange("p (h l) -> p h l", l=64), xsD3[:, :, ls],
                         ALU.add, reads=["ps6", "xsD"], writes=["yv"])
                    k.tt("pool", yb3[:, :, ls], yv3, zs3[:, :, ls], ALU.mult, reads=["yv", "zs"], writes=["yb_blk"])
                    for h in range(4):
                        k.mm(ps[7][:, h * 64:(h + 1) * 64], R(Bdec3[:, h, :]), R(Xdt3[:, h, :]), True, True,
                             reads=["Bdec", "Xdt"], writes=["ps7"])
                    for h in range(4):
                        k.stt("dve", R(S3[:, h, :]), S3[:, h, :], ecsb3[:, h, 63:64], ps[7][:, h * 64:(h + 1) * 64],
                              ALU.mult, ALU.add, reads=["S", "ecsb", "ps7"], writes=["S"])
                k.dma("sp", yb_o[:, :, tok0:tok0 + 512].rearrange("h p t -> p h t"), yb3,
                      reads=["yb_blk"], writes=[("ybo", tok0)])
        P.final_waits("sp")
        P.emit()
    return nc


def p1ab_host(g, c):
    W = g("ab_w_in")[0]
    grp = c // 4
    cols = [np.arange(c * 128, (c + 1) * 128), 1024 + np.arange(c * 128, (c + 1) * 128)]
    zb = 2048
    cols.append(zb + np.arange(256 * c, 256 * (c + 1)))
    xb0 = 2048 + 2048
    cols.append(xb0 + np.arange(256 * c, 256 * (c + 1)))
    cols.append(xb0 + 2048 + np.arange(128 * grp, 128 * (grp + 1)))
    cols.append(xb0 + 2048 + 256 + np.arange(128 * grp, 128 * (grp + 1)))
    cols.append(xb0 + 2560 + np.arange(4 * c, 4 * c + 4))
    cols = np.concatenate(cols)
    w1 = np.ascontiguousarray(W[:, cols])
    rgw = np.ascontiguousarray(np.concatenate([g("rg_wa")[0][c], g("rg_wx")[0][c]], 1))
    pq = np.zeros((128, 48), np.float32)
    sl = slice(c * 128, (c + 1) * 128)
    pq[:, 0:4] = g("rg_conv_w")[0][:, sl].T
    pq[:, 4] = g("rg_conv_b")[0][sl]
    pq[:, 5] = g("rg_ba")[0][sl]
    pq[:, 6] = g("rg_bx")[0][sl]
    pq[:, 7] = g("rg_lam")[0][sl]
    cw = g("ssd_conv_w")[0]
    cb = g("ssd_conv_b")[0]
    for h in range(4):
        hs = slice(256 * c + 64 * h, 256 * c + 64 * (h + 1))
        pq[0:64, 8 + 5 * h:12 + 5 * h] = cw[:, hs].T
        pq[0:64, 12 + 5 * h] = cb[hs]
        pq[0:64, 38 + h] = g("ssd_d")[0][4 * c + h]
    bs = slice(2048 + 128 * grp, 2048 + 128 * (grp + 1))
    cs_ = slice(2048 + 256 + 128 * grp, 2048 + 256 + 128 * (grp + 1))
    pq[:, 28:32] = cw[:, bs].T
    pq[:, 32] = cb[bs]
    pq[:, 33:37] = cw[:, cs_].T
    pq[:, 37] = cb[cs_]
    pq[0:4, 42] = g("ssd_dt_bias")[0][4 * c:4 * c + 4]
    pq[0:4, 43] = g("ssd_a_log")[0][4 * c:4 * c + 4]
    return {"w1": w1, "rgw": rgw, "pq": pq, "cst": ab_cst()}


def ab_cst():
    cst = np.zeros((128, 1408), np.float32)
    cst[:, 0:128] = 1.0
    cst[:, 128:256] = np.eye(128, dtype=np.float32)
    s = np.arange(64)[:, None]
    l = np.arange(64)[None, :]
    cst[0:64, 256:320] = (s <= l).astype(np.float32)
    for h in range(4):
        cst[h, 320 + h * 128:320 + (h + 1) * 128] = 1.0
    rm = np.ones(512, np.float32)
    rm[::64] = 0.0
    cst[0:4, 832:1344] = rm[None, :]
    return cst

import math

TWO_PI = 2.0 * math.pi


def build_p1cd(nbatch=2, nkb=8, do_s5=True, do_gdn=True, stage=9):
    nc = bass.Bass("TRN2", target_bir_lowering=False)

    def D(name, shape, kind="ExternalInput"):
        return nc.dram_tensor(name, shape, F32, kind=kind).ap()

    xT = D("xT", [2048, 8192])
    w1d = D("w1", [2048, 642])
    pqd = D("pq", [128, 64])
    cstd = D("cst", [128, 1792])
    lvd = D("lv", [64, 768])
    s5md = D("s5m", [128, 2048])
    nwd = D("nw", [1, 128])
    gs_o = D("gs", [128, 8192], "ExternalOutput")
    yd_o = D("yd", [128, 8192], "ExternalOutput")

    with contextlib.ExitStack() as es:
        def S(name, shape):
            return es.enter_context(nc.sbuf_tensor(name + "_s", shape, F32))

        w1 = S("w1", [128, 16 * 642])
        xb = S("xb", [128, 16 * 512])
        pq = S("pq", [128, 64])
        cst = S("cst", [128, 1792])
        lv = S("lv", [64, 768])
        s5m = S("s5m", [128, 2048])
        nwb = S("nwb", [64, 128])
        sm = S("sm", [128, 64])
        tabs = S("tabs", [128, 16 * 512])
        rhoT = S("rhoT", [128, 4 * 512])
        u_fm = S("u_fm", [128, 512])
        wk = [S(f"wk{i}", [128, 512]) for i in range(8)]
        hr = S("hr", [128, 512])
        hi = S("hi", [128, 512])
        hprev = S("hprev", [128, 8])
        gin = S("gin", [128, 8])
        ys, gt, gout = wk[0], wk[1], wk[2]
        q_ext = S("q_ext", [128, 515])
        k_ext = S("k_ext", [128, 515])
        v_ext = S("v_ext", [128, 515])
        zs = S("zs", [128, 512])
        q_fm = S("q_fm", [128, 512])
        k_fm = S("k_fm", [128, 512])
        v_fm = S("v_fm", [128, 512])
        acc = S("acc", [128, 512])
        g_fm = S("g_fm", [1, 512])
        b_fm = S("b_fm", [1, 512])
        gcs_fm = S("gcs_fm", [1, 512])
        A_all, AT_all, Dm, DmT = (t[0:64, :] for t in (wk[6], wk[7], wk[4], wk[5]))
        Ek = S("Ek", [64, 512])
        EkT = S("EkT", [64, 512])
        X_sb = S("X_sb", [64, 512])
        Z_sb = S("Z_sb", [64, 512])
        qkT_all = S("qkT_all", [64, 512])
        tokc = S("tokc", [64, 8 * 8])
        egcsb = S("egcsb", [128, 512])
        rhs_u = S("rhs_u", [64, 128])
        rhs_w = S("rhs_w", [64, 128])
        kdec = S("kdec", [64, 128])
        u_sb = S("u_sb", [64, 128])
        wT = S("wT", [128, 64])
        qdec = S("qdec", [128, 64])
        vnew = S("vnew", [64, 128])
        o_sb = S("o_sb", [64, 128])
        osq = S("osq", [64, 128])
        S_sb = S("S_sb", [128, 128])
        sqn = hr
        onesR = S("onesR", [128, 128])
        Dd = S("Dd", [64, 512])
        DdT = S("DdT", [64, 512])
        yd_blk = S("yd_blk", [128, 512])
        ps = [es.enter_context(nc.psum_tensor(f"ps{i}", [128, 512], F32)) for i in range(8)]
        qi = es.enter_context(nc.sbuf_tensor("qi_s", [128, 512], mybir.dt.int32))

        ones = cst[:, 0:128]
        ident = cst[:, 128:256]
        iota = cst[:, 256:768]
        mask_le = cst[0:64, 768:832]
        mask_lt = cst[0:64, 832:896]
        mask_gt = cst[0:64, 896:960]
        rmask = cst[0:1, 1024:1536]
        onesrow = cst[0:1, 0:128]
        onecol = cst[:, 0:1]
        w13 = w1[:].rearrange("p (c n) -> p c n", n=642)
        xbv = xb[:].rearrange("p (c t) -> p c t", t=512)
        tab3 = tabs[:].rearrange("p (i t) -> p i t", t=512)
        rho3 = rhoT[:].rearrange("p (i t) -> p i t", t=512)
        c3 = lambda t: t[:, :].rearrange("p (c l) -> p c l", l=64)

        P = Prog(nc)
        k = K(P)
        for q4 in range(4):
            k.dma("qpool", R(w13[:, 4 * q4:4 * q4 + 4, :]),
                  w1d[q4 * 512:(q4 + 1) * 512, :].rearrange("(c p) n -> p c n", p=128), writes=["w1"])
        k.dma("sp", pq[:], pqd[:, :], writes=["pq"])
        k.dma("sp", cst[:], cstd[:, :], writes=["cst"])
        k.dma("sp", lv[:], lvd[:, :], writes=["lv"])
        k.dma("qpool", R(s5m[:]), s5md[:, :], writes=["s5m"])
        k.dma("sp", nwb[:], nwd[0].partition_broadcast(64), writes=["nwb"])

        rr = ["sm"]
        step = sm[:, 0:4]
        ars = sm[:, 4:8]
        th = sm[:, 8:12]
        rho = sm[:, 12:16]
        cth = sm[:, 16:20]
        sth = sm[:, 20:24]
        lbr = sm[:, 24:28]
        lbi = sm[:, 28:32]
        den = sm[:, 32:36]
        fre = sm[:, 36:40]
        fim = sm[:, 40:44]
        t4a = sm[:, 44:48]
        t4b = sm[:, 48:52]
        nfre = sm[:, 52:56]
        Aneg = sm[0:1, 56:57]

        def sin_to(out, ang, shift, n, rres, wres):
            qf = wk[3][:, 0:n]
            tmp = wk[2][:, 0:n]
            qiv = qi[:, 0:n]
            rq = ["wk3", "wk2", "qi"]
            k.ts("dve", qf, ang, shift, 1.0 / TWO_PI, ALU.add, ALU.mult, reads=rres, writes=rq)
            k.copy("dve", qiv, qf, reads=rq, writes=rq)
            k.copy("dve", qf, qiv, reads=rq, writes=rq)
            k.ts("dve", out, ang, shift, None, ALU.add, reads=rres, writes=wres)
            k.stt("dve", out, qf, -TWO_PI, out, ALU.mult, ALU.add, reads=rq + wres, writes=wres)
            k.ts("dve", tmp, out, math.pi, -TWO_PI, ALU.is_gt, ALU.mult, reads=wres, writes=rq)
            k.tt("dve", out, out, tmp, ALU.add, reads=rq + wres, writes=wres)
            k.ts("dve", tmp, out, -math.pi, TWO_PI, ALU.is_lt, ALU.mult, reads=wres, writes=rq)
            k.tt("dve", out, out, tmp, ALU.add, reads=rq + wres, writes=wres)
            k.ts("dve", out, out, math.pi, -math.pi, ALU.min, ALU.max, reads=wres, writes=wres)
            k.act(out, out, AF.Sin, reads=wres, writes=wres)

        if do_s5:
            k.act(step, pq[:, 8:12], AF.Exp, reads=["pq"], writes=rr)
            k.tt("dve", ars, pq[:, 0:4], step, ALU.mult, reads=rr + ["pq"], writes=rr)
            k.tt("dve", th, pq[:, 4:8], step, ALU.mult, reads=rr + ["pq"], writes=rr)
            k.act(rho, ars, AF.Exp, reads=rr, writes=rr)
            sin_to(cth, th, math.pi / 2, 4, rr, rr)
            sin_to(sth, th, 0.0, 4, rr, rr)
            k.tt("dve", lbr, rho, cth, ALU.mult, reads=rr, writes=rr)
            k.tt("dve", lbi, rho, sth, ALU.mult, reads=rr, writes=rr)
            k.tt("dve", den, pq[:, 0:4], pq[:, 0:4], ALU.mult, reads=rr + ["pq"], writes=rr)
            k.tt("dve", t4a, pq[:, 4:8], pq[:, 4:8], ALU.mult, reads=rr + ["pq"], writes=rr)
            k.tt("dve", den, den, t4a, ALU.add, reads=rr, writes=rr)
            k.P.op("dve", lambda e: e.reciprocal(den, den), reads=rr, writes=rr)
            k.ts("dve", t4a, lbr, -1.0, None, ALU.add, reads=rr, writes=rr)
            k.tt("dve", fre, t4a, pq[:, 0:4], ALU.mult, reads=rr + ["pq"], writes=rr)
            k.tt("dve", t4b, lbi, pq[:, 4:8], ALU.mult, reads=rr + ["pq"], writes=rr)
            k.tt("dve", fre, fre, t4b, ALU.add, reads=rr, writes=rr)
            k.tt("dve", fre, fre, den, ALU.mult, reads=rr, writes=rr)
            k.tt("dve", fim, lbi, pq[:, 0:4], ALU.mult, reads=rr + ["pq"], writes=rr)
            k.tt("dve", t4b, t4a, pq[:, 4:8], ALU.mult, reads=rr + ["pq"], writes=rr)
            k.tt("dve", fim, fim, t4b, ALU.subtract, reads=rr, writes=rr)
            k.tt("dve", fim, fim, den, ALU.mult, reads=rr, writes=rr)
            k.ts("dve", nfre, fre, -1.0, None, ALU.mult, reads=rr, writes=rr)
            for i in range(4):
                Fr, Fi, Cr, Ci = (tab3[:, 4 * i + j, :] for j in range(4))
                ang = wk[0][:]
                k.ts("dve", ang, iota, th[:, i:i + 1], None, ALU.mult, reads=rr + ["cst"], writes=["wk0"])
                sin_to(Cr, ang, math.pi / 2, 512, ["wk0"], ["tabs"])
                sin_to(Ci, ang, 0.0, 512, ["wk0"], ["tabs"])
                k.ts("dve", Fr, Cr, fre[:, i:i + 1], None, ALU.mult, reads=rr + ["tabs"], writes=["tabs"])
                k.stt("dve", Fr, Ci, fim[:, i:i + 1], Fr, ALU.mult, ALU.add, reads=rr + ["tabs"], writes=["tabs"])
                k.ts("dve", Fi, Cr, fim[:, i:i + 1], None, ALU.mult, reads=rr + ["tabs"], writes=["tabs"])
                k.stt("dve", Fi, Ci, nfre[:, i:i + 1], Fi, ALU.mult, ALU.add, reads=rr + ["tabs"], writes=["tabs"])
                k.act(rho3[:, i, :], iota, AF.Identity, scale=0.0, bias=rho[:, i:i + 1],
                      reads=rr + ["cst"], writes=["rhoT"])
        k.memset("dve", sm[:, 61:62], 1e-6, writes=["eps6"])
        k.ts("dve", R(onesR[:]), cst[:, 0:128], 1.0, None, ALU.mult, reads=["cst"], writes=["onesR"])
        if do_gdn:
            k.act(Aneg, pq[0:1, 28:29], AF.Exp, reads=["pq"], writes=["Aneg"])
            k.ts("dve", Aneg, Aneg, -1.0, None, ALU.mult, reads=["Aneg"], writes=["Aneg"])

        for b in range(nbatch):
            k.memset("dve", hprev[:], 0.0, writes=["hprev"])
            k.memset("dve", q_ext[:, 0:3], 0.0, writes=["q_ext"])
            k.memset("dve", k_ext[:, 0:3], 0.0, writes=["k_ext"])
            k.memset("dve", v_ext[:, 0:3], 0.0, writes=["v_ext"])
            k.ts("dve", R(S_sb[:]), cst[:, 0:128], 0.0, None, ALU.mult, reads=["cst"], writes=["S"])
            for kb in range(nkb):
                tok0 = b * 4096 + kb * 512
                for q4 in range(4):
                    k.dma("qpool", R(xbv[:, 4 * q4:4 * q4 + 4, :]),
                          xT[q4 * 512:(q4 + 1) * 512, tok0:tok0 + 512].rearrange("(c p) t -> p c t", p=128),
                          writes=["xb"])
                pcount = [0]

                def proj(col0, M):
                    pn = pcount[0] % 2
                    pcount[0] += 1
                    for kc in range(16):
                        k.mm(ps[pn][0:M, :], R(w13[:, kc, col0:col0 + M]), R(xbv[:, kc, :]),
                             kc == 0, kc == 15, reads=["w1", "xb"], writes=[f"ps{pn}"])
                    return ps[pn], f"ps{pn}"

                if do_s5:
                    pt, pr = proj(0, 128)
                    k.copy("act", R(u_fm[:]), pt[:], reads=[pr], writes=["u_fm"])
                    for i in range(4):
                        Fr, Fi, Cr, Ci = (tab3[:, 4 * i + j, :] for j in range(4))
                        k.mm(ps[2][:], R(s5m[:, i * 128:(i + 1) * 128]), R(u_fm[:]), True, True,
                             reads=["s5m", "u_fm"], writes=["ps2"])
                        k.mm(ps[3][:], R(s5m[:, 512 + i * 128:512 + (i + 1) * 128]), R(u_fm[:]), True, True,
                             reads=["s5m", "u_fm"], writes=["ps3"])
                        k.tt("dve", wk[0][:], ps[2][:], Fr, ALU.mult, reads=["ps2", "tabs"], writes=["wk0"])
                        k.tt("dve", wk[1][:], ps[3][:], Fi, ALU.mult, reads=["ps3", "tabs"], writes=["wk1"])
                        k.tt("pool", wk[0][:], wk[0][:], wk[1][:], ALU.subtract, reads=["wk0", "wk1"], writes=["wk0"])
                        k.tt("dve", wk[2][:], ps[2][:], Fi, ALU.mult, reads=["ps2", "tabs"], writes=["wk2"])
                        k.tt("dve", wk[3][:], ps[3][:], Fr, ALU.mult, reads=["ps3", "tabs"], writes=["wk3"])
                        k.tt("pool", wk[2][:], wk[2][:], wk[3][:], ALU.add, reads=["wk2", "wk3"], writes=["wk2"])
                        hpr = hprev[:, 2 * i:2 * i + 1]
                        hpi = hprev[:, 2 * i + 1:2 * i + 2]
                        gr0 = gin[:, 2 * i:2 * i + 1]
                        gi0 = gin[:, 2 * i + 1:2 * i + 2]
                        tA = gin[:, 0:1] if False else sm[:, 60:61]
                        rg = ["gin"]
                        k.tt("dve", gr0, hpr, cth[:, i:i + 1], ALU.mult, reads=["hprev", "sm"], writes=rg)
                        k.tt("dve", tA, hpi, sth[:, i:i + 1], ALU.mult, reads=["hprev", "sm"], writes=["tA"])
                        k.tt("dve", gr0, gr0, tA, ALU.subtract, reads=rg + ["tA"], writes=rg)
                        k.tt("dve", gi0, hpi, cth[:, i:i + 1], ALU.mult, reads=["hprev", "sm"], writes=rg)
                        k.tt("dve", tA, hpr, sth[:, i:i + 1], ALU.mult, reads=["hprev", "sm"], writes=["tA"])
                        k.tt("dve", gi0, gi0, tA, ALU.add, reads=rg + ["tA"], writes=rg)
                        k.scan(wk[4][:], rho3[:, i, :], wk[0][:], gr0, ALU.mult, ALU.add,
                               reads=["rhoT", "wk0"] + rg, writes=["wk4"])
                        k.scan(wk[5][:], rho3[:, i, :], wk[2][:], gi0, ALU.mult, ALU.add,
                               reads=["rhoT", "wk2"] + rg, writes=["wk5"])
                        k.tt("pool", wk[6][:], wk[4][:], Cr, ALU.mult, reads=["wk4", "tabs"], writes=["wk6"])
                        k.tt("pool", wk[7][:], wk[5][:], Ci, ALU.mult, reads=["wk5", "tabs"], writes=["wk7"])
                        k.tt("dve", R(hr[:]), wk[6][:], wk[7][:], ALU.subtract, reads=["wk6", "wk7"], writes=["hr"])
                        k.tt("pool", wk[6][:], wk[5][:], Cr, ALU.mult, reads=["wk5", "tabs"], writes=["wk6"])
                        k.tt("pool", wk[7][:], wk[4][:], Ci, ALU.mult, reads=["wk4", "tabs"], writes=["wk7"])
                        k.tt("dve", R(hi[:]), wk[6][:], wk[7][:], ALU.add, reads=["wk6", "wk7"], writes=["hi"])
                        k.copy("pool", hpr, hr[:, 511:512], reads=["hr"], writes=["hprev"])
                        k.copy("pool", hpi, hi[:, 511:512], reads=["hi"], writes=["hprev"])
                        k.mm(ps[4][:], R(s5m[:, 1024 + i * 128:1024 + (i + 1) * 128]), R(hr[:]), i == 0, i == 3,
                             reads=["s5m", "hr"], writes=["ps4"])
                        k.mm(ps[5][:], R(s5m[:, 1536 + i * 128:1536 + (i + 1) * 128]), R(hi[:]), i == 0, i == 3,
                             reads=["s5m", "hi"], writes=["ps5"])
                    k.copy("act", ys[:], ps[5][:], reads=["ps5"], writes=["wk0"])
                    k.tt("dve", ys[:], ps[4][:], ys[:], ALU.subtract, reads=["ps4", "wk0"], writes=["wk0"])
                    k.stt("dve", ys[:], u_fm[:], pq[:, 12:13], ys[:], ALU.mult, ALU.add,
                          reads=["u_fm", "pq", "wk0"], writes=["wk0"])
                    gelu_ops(k, gout[:], ys[:], gt[:], "wk0", "wk2", "wk1")
                    k.dma("sp", gs_o[:, tok0:tok0 + 512], gout[:], reads=["wk2"], writes=[("gso", tok0)])

                if not do_gdn:
                    continue
                for (ext, col, nm) in ((q_ext, 128, "q_ext"), (k_ext, 256, "k_ext"), (v_ext, 384, "v_ext")):
                    pt, pr = proj(col, 128)
                    k.copy("act", ext[:, 3:515], pt[:], reads=[pr], writes=[nm])
                pt, pr = proj(512, 128)
                k.act(zs[:], pt[:], AF.Silu, reads=[pr], writes=["zs"])
                pt, pr = proj(640, 1)
                k.act(g_fm[:], pt[0:1, :], AF.Exp, bias=pq[0:1, 29:30], reads=[pr, "pq"], writes=["g_fm"])
                k.act(g_fm[:], g_fm[:], AF.Ln, bias=onecol[0:1, :], reads=["g_fm", "cst"], writes=["g_fm"])
                k.ts("dve", g_fm[:], g_fm[:], Aneg, None, ALU.mult, reads=["g_fm", "Aneg"], writes=["g_fm"])
                k.scan(gcs_fm[:], rmask, g_fm[:], 0.0, ALU.mult, ALU.add, reads=["g_fm", "cst"], writes=["gcs_fm"])
                pt, pr = proj(641, 1)
                k.act(b_fm[:], pt[0:1, :], AF.Sigmoid, reads=[pr], writes=["b_fm"])
                if stage < 1:
                    continue
                for (ext, fm, c0, nm, fn) in ((q_ext, q_fm, 16, "q_ext", "q_fm"), (k_ext, k_fm, 20, "k_ext", "k_fm"),
                                              (v_ext, v_fm, 24, "v_ext", "v_fm")):
                    conv_ops(k, acc[:], ext[:], pq, c0, nm, "acc")
                    k.act(R(fm[:]), acc[:], AF.Silu, reads=["acc"], writes=[fn])
                    k.copy("pool", ext[:, 0:3], ext[:, 512:515], reads=[nm], writes=[nm])
                for (fm, fn, scl) in ((q_fm, "q_fm", 128.0 ** -0.5), (k_fm, "k_fm", 1.0)):
                    k.act(R(sqn[:]), fm[:], AF.Square, reads=[fn], writes=["hr"])
                    k.mm(ps[2][:], R(onesR[:]), R(sqn[:]), True, True, reads=["hr", "onesR"], writes=["ps2"])
                    k.act(acc[:], ps[2][:], AF.Sqrt, bias=sm[:, 61:62], reads=["ps2", "eps6"], writes=["acc"])
                    k.P.op("dve", lambda e: e.reciprocal(acc[:], acc[:]), reads=["acc"], writes=["acc"])
                    k.stt("dve", R(fm[:]), fm[:], scl, acc[:], ALU.mult, ALU.mult, reads=[fn, "acc"], writes=[fn])
                if stage < 2:
                    continue
                for ch in range(8):
                    ls = slice(ch * 64, ch * 64 + 64)
                    tc_ = tokc[:, 8 * ch:8 * ch + 8]
                    rt = [("tokc", ch)]
                    k.tr(ps[2][0:64, 0:1], gcs_fm[0:1, ls], ident[0:1, 0:1], reads=["gcs_fm", "cst"], writes=["ps2"])
                    k.tr(ps[2][0:64, 1:2], b_fm[0:1, ls], ident[0:1, 0:1], reads=["b_fm", "cst"], writes=["ps2"])
                    k.copy("act", tc_[:, 0:2], ps[2][0:64, 0:2], reads=["ps2"], writes=rt)
                    k.mm(ps[3][:, ls], onesrow, gcs_fm[0:1, ls], True, True, reads=["cst", "gcs_fm"], writes=["ps3"])
                    k.mm(ps[4][0:64, ls], onesrow[:, 0:64], b_fm[0:1, ls], True, True, reads=["cst", "b_fm"], writes=["ps4"])
                    k.ts("dve", Dm[:, ls], ps[3][0:64, ls], tc_[:, 0:1], 0.0, ALU.subtract, ALU.min,
                         reads=["ps3"] + rt, writes=["wk4"])
                    k.ts("dve", DmT[:, ls], ps[3][0:64, ls], tc_[:, 0:1], -1.0, ALU.subtract, ALU.mult,
                         reads=["ps3"] + rt, writes=["wk5"])
                    k.ts("dve", DmT[:, ls], DmT[:, ls], 0.0, None, ALU.min, reads=["wk5"], writes=["wk5"])
                    k.act(tc_[:, 2:3], tc_[:, 0:1], AF.Exp, reads=rt, writes=rt)
                    k.tt("dve", tc_[:, 3:4], ps[3][0:64, ch * 64 + 63:ch * 64 + 64], tc_[:, 0:1], ALU.subtract,
                         reads=["ps3"] + rt, writes=rt)
                    k.act(tc_[:, 3:4], tc_[:, 3:4], AF.Exp, reads=rt, writes=rt)
                    k.tt("dve", tc_[:, 4:5], tc_[:, 1:2], tc_[:, 2:3], ALU.mult, reads=rt, writes=rt)
                    k.mm(ps[5][0:64, ls], R(k_fm[:, ls]), R(k_fm[:, ls]), True, True, reads=["k_fm"], writes=["ps5"])
                    k.mm(ps[6][0:64, ls], R(k_fm[:, ls]), R(q_fm[:, ls]), True, True, reads=["k_fm", "q_fm"], writes=["ps6"])
                if stage < 3:
                    continue
                k.act(egcsb[:], ps[3][:], AF.Exp, reads=["ps3"], writes=["egcsb"])
                k.act(Dm[:], Dm[:], AF.Exp, reads=["wk4"], writes=["wk4"])
                k.act(DmT[:], DmT[:], AF.Exp, reads=["wk5"], writes=["wk5"])
                k.tt("dve", AT_all[:], ps[5][0:64, :], ps[4][0:64, :], ALU.mult, reads=["ps5", "ps4"], writes=["wk7"]) \
                    if False else None
                k.copy("act", acc[0:64, :], ps[4][0:64, :], reads=["ps4"], writes=["acc"])
                k.tt("dve", AT_all[:], ps[5][0:64, :], acc[0:64, :], ALU.mult, reads=["ps5", "acc"], writes=["wk7"])
                k.tt("pool", AT_all[:], AT_all[:], Dm[:], ALU.mult, reads=["wk7", "wk4"], writes=["wk7"])
                k.tt("pool", c3(AT_all), c3(AT_all), mask_lt.unsqueeze(1).to_broadcast([64, 8, 64]), ALU.mult,
                     reads=["wk7", "cst"], writes=["wk7"])
                k.tt("dve", A_all[:], ps[5][0:64, :], DmT[:], ALU.mult, reads=["ps5", "wk5"], writes=["wk6"])
                k.tt("pool", c3(A_all), c3(A_all), mask_gt.unsqueeze(1).to_broadcast([64, 8, 64]), ALU.mult,
                     reads=["wk6", "cst"], writes=["wk6"])
                for ch in range(8):
                    ls = slice(ch * 64, ch * 64 + 64)
                    k.ts("dve", A_all[:, ls], A_all[:, ls], tokc[:, 8 * ch + 1:8 * ch + 2], None, ALU.mult,
                         reads=["wk6", ("tokc", ch)], writes=["wk6"])
                k.tt("dve", R(qkT_all[:]), ps[6][0:64, :], Dm[:], ALU.mult, reads=["ps6", "wk4"], writes=["qkT_all"])
                k.tt("pool", R(c3(qkT_all)), c3(qkT_all), mask_le.unsqueeze(1).to_broadcast([64, 8, 64]), ALU.mult,
                     reads=["qkT_all", "cst"], writes=["qkT_all"])
                if stage < 4:
                    continue
                I8 = ident[0:64, 0:64].unsqueeze(1).to_broadcast([64, 8, 64])
                k.tt("dve", R(c3(Ek)), c3(A_all), lv[:, 0:64].unsqueeze(1).to_broadcast([64, 8, 64]), ALU.mult,
                     reads=["wk6", "lv"], writes=["Ek"])
                k.tt("dve", R(c3(Dd)), I8, c3(Ek), ALU.subtract, reads=["Ek", "cst"], writes=["Dd"])
                k.tt("pool", R(c3(EkT)), c3(AT_all), lv[:, 384:448].unsqueeze(1).to_broadcast([64, 8, 64]), ALU.mult,
                     reads=["wk7", "lv"], writes=["EkT"])
                k.tt("pool", R(c3(DdT)), I8, c3(EkT), ALU.subtract, reads=["EkT", "cst"], writes=["DdT"])
                for lev in range(1, 6):
                    k.tt("dve", R(c3(Ek)), c3(A_all), lv[:, 64 * lev:64 * lev + 64].unsqueeze(1).to_broadcast([64, 8, 64]),
                         ALU.mult, reads=["wk6", "lv"], writes=["Ek"])
                    k.tt("pool", R(c3(EkT)), c3(AT_all),
                         lv[:, 384 + 64 * lev:384 + 64 * lev + 64].unsqueeze(1).to_broadcast([64, 8, 64]),
                         ALU.mult, reads=["wk7", "lv"], writes=["EkT"])
                    for ch in range(8):
                        ls = slice(ch * 64, ch * 64 + 64)
                        k.mm(ps[2][0:64, ls], R(EkT[:, ls]), R(Dd[:, ls]), True, True, reads=["EkT", "Dd"], writes=["ps2"])
                    k.copy("act", R(X_sb[:]), ps[2][0:64, :], reads=["ps2"], writes=["X_sb"])
                    for ch in range(8):
                        ls = slice(ch * 64, ch * 64 + 64)
                        k.mm(ps[4][0:64, ls], R(Ek[:, ls]), R(DdT[:, ls]), True, True, reads=["Ek", "DdT"], writes=["ps4"])
                    k.copy("act", R(Z_sb[:]), ps[4][0:64, :], reads=["ps4"], writes=["Z_sb"])
                    for ch in range(8):
                        ls = slice(ch * 64, ch * 64 + 64)
                        k.mm(ps[5][0:64, ls], R(DdT[:, ls]), R(X_sb[:, ls]), True, True, reads=["DdT", "X_sb"], writes=["ps5"])
                        k.mm(ps[6][0:64, ls], R(Dd[:, ls]), R(Z_sb[:, ls]), True, True, reads=["Dd", "Z_sb"], writes=["ps6"])
                    k.tt("dve", R(Dd[:]), Dd[:], ps[5][0:64, :], ALU.subtract, reads=["Dd", "ps5"], writes=["Dd"])
                    k.tt("dve", R(DdT[:]), DdT[:], ps[6][0:64, :], ALU.subtract, reads=["DdT", "ps6"], writes=["DdT"])
                if stage < 5:
                    continue
                for ch in range(8):
                    ls = slice(ch * 64, ch * 64 + 64)
                    tc_ = tokc[:, 8 * ch:8 * ch + 8]
                    rt = [("tokc", ch)]
                    k.tr(ps[2][0:64, 0:128], k_fm[:, ls], ident, reads=["k_fm", "cst"], writes=["ps2"])
                    k.tr(ps[2][0:64, 128:256], v_fm[:, ls], ident, reads=["v_fm", "cst"], writes=["ps2"])
                    import os
                    SUB = int(os.environ.get("SUB", "9"))
                    if SUB < 1:
                        continue
                    k.ts("dve", R(rhs_w[:]), ps[2][0:64, 0:128], tc_[:, 4:5], None, ALU.mult, reads=["ps2"] + rt, writes=["rhs_w"])
                    if SUB < 2:
                        continue
                    k.ts("dve", R(kdec[:]), ps[2][0:64, 0:128], tc_[:, 3:4], None, ALU.mult, reads=["ps2"] + rt, writes=["kdec"])
                    k.ts("dve", R(rhs_u[:]), ps[2][0:64, 128:256], tc_[:, 1:2], None, ALU.mult, reads=["ps2"] + rt, writes=["rhs_u"])
                    if SUB < 3:
                        continue
                    k.tt("pool", R(qdec[:]), q_fm[:, ls], egcsb[:, ls], ALU.mult, reads=["q_fm", "egcsb"], writes=["qdec"])
                    if stage < 6:
                        continue
                    k.mm(ps[7][0:64, 0:128], R(DdT[:, ls]), R(rhs_u[:]), True, True, reads=["DdT", "rhs_u"], writes=["ps7"])
                    k.mm(ps[7][:, 128:192], R(rhs_w[:]), R(DdT[:, ls]), True, True, reads=["DdT", "rhs_w"], writes=["ps7"])
                    k.copy("act", u_sb[:], ps[7][0:64, 0:128], reads=["ps7"], writes=["u_sb"])
                    k.copy("dve", R(wT[:]), ps[7][:, 128:192], reads=["ps7"], writes=["wT"])
                    if stage < 7:
                        continue
                    k.mm(ps[7][0:64, 256:384], R(wT[:]), R(S_sb[:]), True, True, reads=["wT", "S"], writes=["ps7"])
                    k.tt("dve", R(vnew[:]), u_sb[:], ps[7][0:64, 256:384], ALU.subtract, reads=["u_sb", "ps7"], writes=["vnew"])
                    k.mm(ps[3][0:64, 0:128], R(qdec[:]), R(S_sb[:]), True, False, reads=["qdec", "S"], writes=["ps3"])
                    k.mm(ps[3][0:64, 0:128], R(qkT_all[:, ls]), R(vnew[:]), False, True, reads=["qkT_all", "vnew"], writes=["ps3"])
                    k.mm(ps[7][:, 384:512], R(kdec[:]), R(vnew[:]), True, True, reads=["kdec", "vnew"], writes=["ps7"])
                    k.stt("dve", R(S_sb[:]), S_sb[:], egcsb[:, ch * 64 + 63:ch * 64 + 64], ps[7][:, 384:512], ALU.mult, ALU.add,
                          reads=["S", "egcsb", "ps7"], writes=["S"])
                    if stage < 8:
                        continue
                    k.copy("act", o_sb[:], ps[3][0:64, 0:128], reads=["ps3"], writes=["o_sb"])
                    k.act(osq[:], o_sb[:], AF.Square, accum_out=tc_[:, 5:6], reads=["o_sb"], writes=["osq"] + rt)
                    k.act(tc_[:, 5:6], tc_[:, 5:6], AF.Sqrt, scale=1.0 / 128, bias=sm[0:64, 61:62],
                          reads=rt + ["eps6"], writes=rt)
                    k.P.op("dve", lambda e, a=tc_[:, 5:6]: e.reciprocal(a, a), reads=rt, writes=rt)
                    k.stt("dve", o_sb[:], o_sb[:], tc_[:, 5:6], nwb[:], ALU.mult, ALU.mult, reads=["o_sb", "nwb"] + rt, writes=["o_sb"])
                    k.tr(ps[3][:, 128:192], o_sb[:], ident[0:64, 0:64], reads=["o_sb", "cst"], writes=["ps3"])
                    k.tt("dve", yd_blk[:, ls], ps[3][:, 128:192], zs[:, ls], ALU.mult, reads=["ps3", "zs"], writes=["yd_blk"])
                k.dma("sp", yd_o[:, tok0:tok0 + 512], yd_blk[:], reads=["yd_blk"], writes=[("ydo", tok0)])
        P.final_waits("sp")
        P.emit()
    return nc


def cd_cst():
    cst = np.zeros((128, 1792), np.float32)
    cst[:, 0:128] = 1.0
    cst[:, 128:256] = np.eye(128, dtype=np.float32)
    cst[:, 256:768] = np.arange(512, dtype=np.float32)[None, :]
    s = np.arange(64)[:, None]
    l = np.arange(64)[None, :]
    cst[0:64, 768:832] = (s <= l)
    cst[0:64, 832:896] = (s < l)
    cst[0:64, 896:960] = (s > l)
    rm = np.ones(512, np.float32)
    rm[::64] = 0.0
    cst[0:1, 1024:1536] = rm[None, :]
    lv = np.zeros((64, 768), np.float32)
    i = np.arange(64)[:, None]
    j = np.arange(64)[None, :]
    for kk in range(6):
        bsz = 2 ** kk
        m = ((i // (2 * bsz)) == (j // (2 * bsz))) & ((i % (2 * bsz)) >= bsz) & ((j % (2 * bsz)) < bsz)
        lv[:, 64 * kk:64 * kk + 64] = m
        lv[:, 384 + 64 * kk:384 + 64 * kk + 64] = m.T
    return cst, lv


def p1cd_host(g, c):
    W = g("cd_w_in")[0]
    cols = np.concatenate([np.arange(c * 128, (c + 1) * 128), 1024 + np.arange(c * 128, (c + 1) * 128),
                           2048 + np.arange(c * 128, (c + 1) * 128), 3072 + np.arange(c * 128, (c + 1) * 128),
                           4096 + np.arange(c * 128, (c + 1) * 128), [5120 + c], [5128 + c]])
    w1 = np.ascontiguousarray(W[:, cols])
    pq = np.zeros((128, 64), np.float32)
    are, aim, ls_ = g("s5_a_re")[0], g("s5_a_im")[0], g("s5_log_step")[0]
    s5m = np.zeros((128, 2048), np.float32)
    bre, bim, cre, cim = g("s5_b_re")[0], g("s5_b_im")[0], g("s5_c_re")[0], g("s5_c_im")[0]
    for i in range(4):
        for gl in range(2):
            gg = 8 * c + 2 * i + gl
            pq[gl * 64:(gl + 1) * 64, i] = are[gg]
            pq[gl * 64:(gl + 1) * 64, 4 + i] = aim[gg]
            pq[gl * 64:(gl + 1) * 64, 8 + i] = ls_[gg]
            chs = slice(32 * i + 16 * gl, 32 * i + 16 * gl + 16)
            sts = slice(i * 128 + gl * 64, i * 128 + gl * 64 + 64)
            s5m[chs, sts] = bre[gg].T
            s5m[chs, 512 + i * 128 + gl * 64:512 + i * 128 + gl * 64 + 64] = bim[gg].T
            s5m[gl * 64:(gl + 1) * 64, 1024 + i * 128 + 32 * i + 16 * gl:1024 + i * 128 + 32 * i + 16 * gl + 16] = cre[gg].T
            s5m[gl * 64:(gl + 1) * 64, 1536 + i * 128 + 32 * i + 16 * gl:1536 + i * 128 + 32 * i + 16 * gl + 16] = cim[gg].T
    pq[:, 12] = g("s5_d")[0][c * 128:(c + 1) * 128]
    cw = g("dn_conv_w")[0]
    pq[:, 16:20] = cw[:, c * 128:(c + 1) * 128].T
    pq[:, 20:24] = cw[:, 1024 + c * 128:1024 + (c + 1) * 128].T
    pq[:, 24:28] = cw[:, 2048 + c * 128:2048 + (c + 1) * 128].T
    pq[0, 28] = g("dn_a_log")[0][c]
    pq[0, 29] = g("dn_dt_bias")[0][c]
    cst, lv = cd_cst()
    return {"w1": w1, "pq": pq, "cst": cst, "lv": lv, "s5m": s5m, "nw": g("dn_norm_w")[0][None, :].astype(np.float32)}


HD_SCALE = 512 ** -0.5
BIG = 1.0e9


def build_p2(kind, nblk=2, do_moe=True, do_xattn=True):
    ab = kind == "ab"
    KY = 3072 if ab else 2048
    KYC = KY // 128
    nc = bass.Bass("TRN2", target_bir_lowering=False)

    def D(name, shape, kind="ExternalInput"):
        return nc.dram_tensor(name, shape, F32, kind=kind).ap()

    xT = D("xT", [2048, 1024])
    yT = D("yT", [KY, 1024])
    memT = D("memT", [2048, 256])
    w_out = D("w_out", [KY, 2048])
    w_q = D("w_q", [2048, 2048])
    w_kv = D("w_kv", [2048, 4096])
    w_o = D("w_o", [2048, 2048])
    w_r = D("w_r", [2048, 36])
    b_r = D("b_r", [1, 36])
    w_gate = D("w_gate", [32, 2048, 256])
    w_up = D("w_up", [32, 2048, 256])
    w_down = D("w_down", [32, 256, 2048])
    pp_d = D("pp", [128, 112])
    cst_d = D("cst", [128, 256])
    if not ab:
        glu_w = D("glu_w", [1024, 1024])
    xo = D("xo", [2048, 1024], "ExternalOutput")

    with contextlib.ExitStack() as es:
        def S(name, shape):
            return es.enter_context(nc.sbuf_tensor(name + "_s", shape, F32))

        x_sb = S("x_sb", [128, 16 * 512])
        bufY = S("bufY", [128, 24 * 512])
        V_sb = S("V_sb", [128, 2 * 2048])
        ring = [S(f"ring{i}", [128, 4096]) for i in range(3)]
        q_sb = S("q_sb", [128, 4 * 512])
        e_sb = S("e_sb", [128, 4 * 256])
        pT_sb = S("pT_sb", [128, 2 * 512])
        sqt = [S(f"sq{i}", [128, 512]) for i in range(2)]
        t1t = [S(f"t1_{i}", [128, 512]) for i in range(2)]
        t2t = [S(f"t2_{i}", [128, 512]) for i in range(2)]
        h_sb = [S(f"h{i}", [128, 2 * 512]) for i in range(2)]
        cbc = [S(f"cbc{i}", [128, 512]) for i in range(2)]
        mean = S("mean", [128, 512])
        msq = S("msq", [128, 512])
        rstd = S("rstd", [128, 512])
        nmr = S("nmr", [128, 512])
        pp = S("pp_sb", [128, 112])
        cst = S("cst_sb", [128, 256])
        wr_sb = S("wr_sb", [128, 16 * 36])
        brb = S("brb", [128, 36])
        combT = S("combT", [32, 512])
        selb = [S(f"sel{i}", [32, 128]) for i in range(2)]
        sm = S("sm", [128, 256])
        ps = [es.enter_context(nc.psum_tensor(f"ps{i}", [128, 512], F32)) for i in range(8)]

        ones = cst[:, 0:128]
        ident = cst[:, 128:256]
        x3 = x_sb[:].rearrange("p (c t) -> p c t", t=512)
        y3 = bufY[:].rearrange("p (c t) -> p c t", t=512)
        kT3 = bufY[:, 16 * 512:24 * 512].rearrange("p (c m) -> p c m", m=256)
        mem3 = bufY[:, 0:4096].rearrange("p (c m) -> p c m", m=256)
        V3 = V_sb[:].rearrange("p (mc d) -> p mc d", d=2048)
        q3 = q_sb[:].rearrange("p (c t) -> p c t", t=512)
        e3 = e_sb[:].rearrange("p (tt m) -> p tt m", m=256)
        pT3 = pT_sb[:].rearrange("p (mc t) -> p mc t", t=512)
        wr3 = wr_sb[:].rearrange("p (c n) -> p c n", n=36)

        P = Prog(nc)
        k = K(P)
        state = {"slot": 0}

        def wslot():
            i = state["slot"] % 3
            state["slot"] += 1
            return ring[i], f"ring{i}"

        def wload(src_ap, kc, ncol):
            t, rn = wslot()
            v = t[:, 0:kc * ncol].rearrange("p (c j) -> p c j", j=ncol)
            k.dma("qpool", R(v), src_ap.rearrange("(c p) j -> p c j", p=128), writes=[rn])
            return v, rn

        epsb = S("epsb", [128, 2])
        k.memset("dve", epsb[:, 0:1], LN_EPS, writes=["epsb"])
        k.memset("dve", epsb[:, 1:2], RMS_EPS, writes=["epsb"])
        k.dma("sp", pp[:], pp_d[:, :], writes=["pp"])
        k.dma("qpool", R(cst[:]), cst_d[:, :], writes=["cst"])
        k.dma("sp", wr3, w_r.rearrange("(c p) n -> p c n", p=128), writes=["wr"])
        k.dma("sp", brb[:], b_r[0].partition_broadcast(128), writes=["brb"])

        def layer_norm(g0, b0):
            for j in range(16):
                k.mm(ps[3][:], R(ones), R(x3[:, j, :]), j == 0, j == 15,
                     reads=[("x", j), "cst"], writes=["ps3"])
            for j in range(16):
                sq = sqt[j % 2]
                k.act(R(sq[:]), x3[:, j, :], AF.Square, reads=[("x", j)], writes=[("sq", j % 2)])
                k.mm(ps[4][:], R(ones), R(sq[:]), j == 0, j == 15,
                     reads=[("sq", j % 2), "cst"], writes=["ps4"])
            k.act(mean[:], ps[3][:], AF.Copy, scale=1.0 / 2048, reads=["ps3"], writes=["mean"])
            k.act(msq[:], ps[3][:], AF.Square, scale=1.0 / 2048, reads=["ps3"], writes=["msq"])
            k.stt("dve", rstd[:], ps[4][:], 1.0 / 2048, msq[:], ALU.mult, ALU.subtract,
                  reads=["ps4", "msq"], writes=["rstd"])
            k.act(rstd[:], rstd[:], AF.Sqrt, bias=epsb[:, 0:1], reads=["rstd", "epsb"], writes=["rstd"])
            k.P.op("dve", lambda e: e.reciprocal(rstd[:], rstd[:]), reads=["rstd"], writes=["rstd"])
            k.stt("dve", nmr[:], mean[:], -1.0, rstd[:], ALU.mult, ALU.mult,
                  reads=["mean", "rstd"], writes=["nmr"])
            for j in range(16):
                t1 = t1t[j % 2]
                t2 = t2t[j % 2]
                k.tt("dve", t1[:], x3[:, j, :], rstd[:], ALU.mult,
                     reads=[("x", j), "rstd"], writes=[("t1", j % 2)])
                k.tt("pool", t2[:], t1[:], nmr[:], ALU.add,
                     reads=[("t1", j % 2), "nmr"], writes=[("t2", j % 2)])
                k.act(R(x3[:, j, :]), t2[:], AF.Identity, scale=pp[:, g0 + j:g0 + j + 1],
                      bias=pp[:, b0 + j:b0 + j + 1], reads=[("t2", j % 2), "pp"], writes=[("x", j)])

        def proj_residual(w_dram, kyc, rhs_of, rhs_res_of):
            ncol = 128 if kyc == 24 else 256
            per = ncol // 128
            for jt in range(2048 // ncol):
                wv, rn = wload(w_dram[:, jt * ncol:(jt + 1) * ncol], kyc, ncol)
                for jj in range(per):
                    j = jt * per + jj
                    pn = 1 + j % 2
                    for kc in range(kyc):
                        k.mm(ps[pn][:], R(wv[:, kc, jj * 128:(jj + 1) * 128]), R(rhs_of(kc)),
                             kc == 0, kc == kyc - 1, reads=[rn, rhs_res_of(kc)], writes=[f"ps{pn}"])
                    k.stt("dve", R(x3[:, j, :]), x3[:, j, :], ALPHA, ps[pn][:], ALU.mult, ALU.add,
                          reads=[("x", j), f"ps{pn}"], writes=[("x", j)])

        for blk in range(nblk):
            t0 = blk * 512
            for q4 in range(4):
                k.dma("qpool", R(x3[:, 4 * q4:4 * q4 + 4, :]),
                      xT[q4 * 512:(q4 + 1) * 512, t0:t0 + 512].rearrange("(c p) t -> p c t", p=128),
                      writes=[("x", 4 * q4 + i) for i in range(4)])
            for q4 in range(KYC // 4):
                k.dma("qpool", R(y3[:, 4 * q4:4 * q4 + 4, :]),
                      yT[q4 * 512:(q4 + 1) * 512, t0:t0 + 512].rearrange("(c p) t -> p c t", p=128),
                      writes=[("y", 4 * q4 + i) for i in range(4)])
            if ab:
                for g in range(2):
                    for i in range(8):
                        j = 8 + 8 * g + i
                        sq = sqt[i % 2]
                        k.act(R(sq[:]), y3[:, j, :], AF.Square, reads=[("y", j)], writes=[("sq", i % 2)])
                        k.mm(ps[0][:], R(ones), R(sq[:]), i == 0, i == 7,
                             reads=[("sq", i % 2), "cst"], writes=["ps0"])
                    k.act(rstd[:], ps[0][:], AF.Sqrt, scale=1.0 / 1024, bias=epsb[:, 1:2],
                          reads=["ps0", "epsb"], writes=["rstd"])
                    k.P.op("dve", lambda e: e.reciprocal(rstd[:], rstd[:]), reads=["rstd"], writes=["rstd"])
                    for i in range(8):
                        j = 8 + 8 * g + i
                        k.stt("dve", R(y3[:, j, :]), y3[:, j, :], pp[:, 96 + j - 8:97 + j - 8], rstd[:],
                              ALU.mult, ALU.mult, reads=[("y", j), "rstd", "pp"], writes=[("y", j)])
                rhs_of = lambda kc: y3[:, kc, :]
                rhs_res_of = lambda kc: ("y", kc)
            else:
                for j2 in range(4):
                    wv, rn = wload(glu_w[:, j2 * 256:(j2 + 1) * 256], 8, 256)
                    for jj in range(2):
                        j = 2 * j2 + jj
                        pn = 5 + jj
                        for kc in range(8):
                            k.mm(ps[pn][:], R(wv[:, kc, jj * 128:(jj + 1) * 128]), R(y3[:, kc, :]),
                                 kc == 0, kc == 7, reads=[rn, ("y", kc)], writes=[f"ps{pn}"])
                        t1 = t1t[j % 2]
                        k.act(t1[:], ps[pn][:], AF.Sigmoid, bias=pp[:, 96 + j:97 + j],
                              reads=[f"ps{pn}", "pp"], writes=[("t1", j % 2)])
                        k.tt("dve", R(y3[:, 16 + j, :]), t1[:], y3[:, j, :], ALU.mult,
                             reads=[("t1", j % 2), ("y", j)], writes=[("y", 16 + j)])
                rhs_of = lambda kc: y3[:, 16 + kc, :] if kc < 8 else y3[:, kc, :]
                rhs_res_of = lambda kc: ("y", 16 + kc) if kc < 8 else ("y", kc)
            proj_residual(w_out, KYC, rhs_of, rhs_res_of)
            layer_norm(0, 16)

            if do_xattn:
                for q4 in range(4):
                    k.dma("qpool", R(mem3[:, 4 * q4:4 * q4 + 4, :]),
                          memT[q4 * 512:(q4 + 1) * 512, :].rearrange("(c p) m -> p c m", p=128),
                          writes=[("y", 2 * q4), ("y", 2 * q4 + 1)])
                memres = [("y", i) for i in range(8)]
                for kt in range(8):
                    wv, rn = wload(w_kv[:, kt * 256:(kt + 1) * 256], 16, 256)
                    for jj in range(2):
                        c = 2 * kt + jj
                        pn = c % 4
                        for kc in range(16):
                            k.mm(ps[pn][:, 0:256], R(wv[:, kc, jj * 128:(jj + 1) * 128]), R(mem3[:, kc, :]),
                                 kc == 0, kc == 15, reads=[rn, ("y", kc // 2)], writes=[f"ps{pn}"])
                        k.copy("act", R(kT3[:, c, :]), ps[pn][:, 0:256],
                               reads=[f"ps{pn}"], writes=[("y", 16 + c // 2)])
                if blk == 0:
                    for vt in range(8):
                        wv, rn = wload(w_kv[:, 2048 + vt * 256:2048 + (vt + 1) * 256], 16, 256)
                        for mc in range(2):
                            pn = (2 * vt + mc) % 4
                            for kc in range(16):
                                k.mm(ps[pn][:, 0:256], R(mem3[:, kc, mc * 128:(mc + 1) * 128]), R(wv[:, kc, :]),
                                     kc == 0, kc == 15, reads=[rn, ("y", kc // 2)], writes=[f"ps{pn}"])
                            k.copy("dve", R(V3[:, mc, vt * 256:(vt + 1) * 256]), ps[pn][:, 0:256],
                                   reads=[f"ps{pn}"], writes=["V"])
                for h in range(4):
                    for qt in range(2):
                        wv, rn = wload(w_q[:, h * 512 + qt * 256:h * 512 + (qt + 1) * 256], 16, 256)
                        for jj in range(2):
                            c = 2 * qt + jj
                            pn = c % 4
                            for kc in range(16):
                                k.mm(ps[pn][:], R(wv[:, kc, jj * 128:(jj + 1) * 128]), R(x3[:, kc, :]),
                                     kc == 0, kc == 15, reads=[rn, ("x", kc)], writes=[f"ps{pn}"])
                            k.act(R(q3[:, c, :]), ps[pn][:], AF.Copy, scale=HD_SCALE,
                                  reads=[f"ps{pn}"], writes=[("q", c)])
                    for tt in range(4):
                        pn = 4 + tt // 2
                        for c in range(4):
                            k.mm(ps[pn][:, (tt % 2) * 256:(tt % 2 + 1) * 256],
                                 R(q3[:, c, tt * 128:(tt + 1) * 128]), R(kT3[:, h * 4 + c, :]),
                                 c == 0, c == 3, reads=[("q", c), ("y", 16 + (h * 4 + c) // 2)],
                                 writes=[f"ps{pn}"])
                    mx = sm[:, 0:4]
                    nmx = sm[:, 4:8]
                    ssum = sm[:, 8:12]
                    rs = sm[:, 12:16]
                    for half in range(2):
                        k.reduce("dve", mx[:, 2 * half:2 * half + 2],
                                 ps[4 + half][:].rearrange("p (a m) -> p a m", m=256), ALU.max,
                                 reads=[f"ps{4 + half}"], writes=["mx"])
                    k.ts("dve", nmx, mx, -1.0, None, ALU.mult, reads=["mx"], writes=["nmx"])
                    for tt in range(4):
                        pn = 4 + tt // 2
                        k.act(e3[:, tt, :], ps[pn][:, (tt % 2) * 256:(tt % 2 + 1) * 256], AF.Exp,
                              bias=nmx[:, tt:tt + 1], accum_out=ssum[:, tt:tt + 1],
                              reads=[f"ps{pn}", "nmx"], writes=[("e", tt), "ssum"])
                    k.P.op("dve", lambda e, rs=rs, ssum=ssum: e.reciprocal(rs, ssum), reads=["ssum"], writes=["rs"])
                    for tt in range(4):
                        k.ts("dve" if tt % 2 == 0 else "pool", e3[:, tt, :], e3[:, tt, :], rs[:, tt:tt + 1], None,
                             ALU.mult, reads=[("e", tt), "rs"], writes=[("e", tt)])
                    for mc in range(2):
                        for tt in range(4):
                            k.tr(ps[6 + mc][:, tt * 128:(tt + 1) * 128], e3[:, tt, mc * 128:(mc + 1) * 128], ident,
                                 reads=[("e", tt), "cst"], writes=[f"ps{6 + mc}"])
                        k.copy("act" if mc == 0 else "dve", R(pT3[:, mc, :]), ps[6 + mc][:],
                               reads=[f"ps{6 + mc}"], writes=[("pT", mc)])
                    for c in range(4):
                        pn = c % 4
                        for mc in range(2):
                            k.mm(ps[pn][:], R(V3[:, mc, (h * 4 + c) * 128:(h * 4 + c + 1) * 128]), R(pT3[:, mc, :]),
                                 mc == 0, mc == 1, reads=["V", ("pT", mc)], writes=[f"ps{pn}"])
                        k.copy("act", R(y3[:, h * 4 + c, :]), ps[pn][:],
                               reads=[f"ps{pn}"], writes=[("y", h * 4 + c)])
                proj_residual(w_o, 16, lambda kc: y3[:, kc, :], lambda kc: ("y", kc))
                layer_norm(32, 48)

            if do_moe:
                for tt in range(4):
                    for kc in range(16):
                        k.mm(ps[0][:, tt * 36:(tt + 1) * 36], x3[:, kc, tt * 128:(tt + 1) * 128], wr3[:, kc, :],
                             kc == 0, kc == 15, reads=[("x", kc), "wr"], writes=["ps0"])
                for tt in range(4):
                    lg = sm[:, 16:52]
                    gl = sm[:, 16:20]
                    el = sm[:, 20:52]
                    gmax = sm[:, 52:53]
                    ngmax = sm[:, 53:54]
                    gsum = sm[:, 54:55]
                    gp = sm[:, 55:56]
                    m1 = sm[:, 56:57]
                    m2 = sm[:, 57:58]
                    d21 = sm[:, 58:59]
                    w1 = sm[:, 59:60]
                    w2 = sm[:, 60:61]
                    ohg = sm[:, 64:68]
                    pen = sm[:, 68:72]
                    tmp4 = sm[:, 72:76]
                    elm = sm[:, 80:112]
                    oh1 = sm[:, 112:144]
                    elm2 = sm[:, 144:176]
                    oh2 = sm[:, 176:208]
                    comb = sm[:, 208:240]
                    rr = ["smr"]
                    k.tt("dve", lg, ps[0][:, tt * 36:(tt + 1) * 36], brb[:], ALU.add,
                         reads=["ps0", "brb"], writes=rr)
                    k.reduce("dve", gmax, gl, ALU.max, reads=rr, writes=rr)
                    k.ts("dve", ohg, gl, gmax, None, ALU.is_equal, reads=rr, writes=rr)
                    k.ts("dve", ngmax, gmax, -1.0, None, ALU.mult, reads=rr, writes=rr)
                    k.act(tmp4, gl, AF.Exp, bias=ngmax, accum_out=gsum, reads=rr, writes=rr)
                    k.P.op("dve", lambda e, gp=gp, gsum=gsum: e.reciprocal(gp, gsum), reads=rr, writes=rr)
                    k.ts("dve", pen, ohg, BIG, -BIG, ALU.mult, ALU.add, reads=rr, writes=rr)
                    k.tt("dve", elm.rearrange("p (g e) -> p g e", e=8), el.rearrange("p (g e) -> p g e", e=8),
                         pen.unsqueeze(2).to_broadcast([128, 4, 8]), ALU.add, reads=rr, writes=rr)
                    k.reduce("dve", m1, elm, ALU.max, reads=rr, writes=rr)
                    k.ts("dve", oh1, elm, m1, None, ALU.is_equal, reads=rr, writes=rr)
                    k.stt("dve", elm2, oh1, -BIG, elm, ALU.mult, ALU.add, reads=rr, writes=rr)
                    k.reduce("dve", m2, elm2, ALU.max, reads=rr, writes=rr)
                    k.ts("dve", oh2, elm2, m2, None, ALU.is_equal, reads=rr, writes=rr)
                    k.tt("dve", d21, m2, m1, ALU.subtract, reads=rr, writes=rr)
                    k.act(w1, d21, AF.Exp, reads=rr, writes=rr)
                    k.ts("dve", w1, w1, 1.0, None, ALU.add, reads=rr, writes=rr)
                    k.P.op("dve", lambda e, w1=w1: e.reciprocal(w1, w1), reads=rr, writes=rr)
                    k.tt("dve", w1, w1, gp, ALU.mult, reads=rr, writes=rr)
                    k.tt("dve", w2, gp, w1, ALU.subtract, reads=rr, writes=rr)
                    k.ts("dve", comb, oh1, w1, None, ALU.mult, reads=rr, writes=rr)
                    k.stt("dve", comb, oh2, w2, comb, ALU.mult, ALU.add, reads=rr, writes=rr)
                    k.tr(ps[1][0:32, tt * 128:(tt + 1) * 128], comb, ident, reads=rr + ["cst"], writes=["ps1"])
                k.copy("act", R(combT[:]), ps[1][0:32, :], reads=["ps1"], writes=["combT"])
                for e in range(32):
                    sb = selb[e % 2]
                    cb = cbc[e % 2]
                    hh = h_sb[e % 2]
                    h3 = hh[:].rearrange("p (f t) -> p f t", t=512)
                    k.copy("dve", R(sb[:]), ident[0:32, e:e + 1].to_broadcast([32, 128]),
                           reads=["cst"], writes=[("sel", e % 2)])
                    k.mm(ps[2][:], R(sb[:]), R(combT[:]), True, True,
                         reads=[("sel", e % 2), "combT"], writes=["ps2"])
                    k.copy("act", cb[:], ps[2][:], reads=["ps2"], writes=[("cbc", e % 2)])
                    wg, rg = wload(w_gate[e], 16, 256)
                    wu, ru = wload(w_up[e], 16, 256)
                    td, rd = wslot()
                    wd = td[:].rearrange("p (f d) -> p f d", d=2048)
                    k.dma("qpool", R(wd), w_down[e].rearrange("(f p) d -> p f d", p=128), writes=[rd])
                    for fc in range(2):
                        gpn = 3 + 2 * fc
                        upn = 4 + 2 * fc
                        for kc in range(16):
                            k.mm(ps[gpn][:], R(wg[:, kc, fc * 128:(fc + 1) * 128]), R(x3[:, kc, :]),
                                 kc == 0, kc == 15, reads=[rg, ("x", kc)], writes=[f"ps{gpn}"])
                        for kc in range(16):
                            k.mm(ps[upn][:], R(wu[:, kc, fc * 128:(fc + 1) * 128]), R(x3[:, kc, :]),
                                 kc == 0, kc == 15, reads=[ru, ("x", kc)], writes=[f"ps{upn}"])
                        t1 = t1t[fc]
                        t2 = t2t[fc]
                        k.act(t1[:], ps[gpn][:], AF.Silu, reads=[f"ps{gpn}"], writes=[("t1", fc)])
                        k.tt("dve", t2[:], t1[:], ps[upn][:], ALU.mult,
                             reads=[("t1", fc), f"ps{upn}"], writes=[("t2", fc)])
                        k.tt("pool", R(h3[:, fc, :]), t2[:], cb[:], ALU.mult,
                             reads=[("t2", fc), ("cbc", e % 2)], writes=[("h", e % 2, fc)])
                    for j in range(16):
                        dn = [7, 0, 1][j % 3]
                        for fc in range(2):
                            k.mm(ps[dn][:], R(wd[:, fc, j * 128:(j + 1) * 128]), R(h3[:, fc, :]),
                                 fc == 0, fc == 1, reads=[rd, ("h", e % 2, fc)], writes=[f"ps{dn}"])
                        if e == 0:
                            k.copy("dve", R(y3[:, j, :]), ps[dn][:], reads=[f"ps{dn}"], writes=[("y", j)])
                        else:
                            k.tt("dve", R(y3[:, j, :]), ps[dn][:], y3[:, j, :], ALU.add,
                                 reads=[f"ps{dn}", ("y", j)], writes=[("y", j)])
                for j in range(16):
                    k.stt("dve", R(x3[:, j, :]), x3[:, j, :], ALPHA, y3[:, j, :], ALU.mult, ALU.add,
                          reads=[("x", j), ("y", j)], writes=[("x", j)])
                layer_norm(64, 80)

            for q4 in range(4):
                k.dma("sp", xo[q4 * 512:(q4 + 1) * 512, t0:t0 + 512].rearrange("(c p) t -> p c t", p=128),
                      x3[:, 4 * q4:4 * q4 + 4, :],
                      reads=[("x", 4 * q4 + i) for i in range(4)], writes=[("xo", blk, q4)])
        P.final_waits("sp")
        P.emit()
    return nc

from concourse.bass_utils import run_bass_kernel_spmd

_CACHE = {}


def _chunkpack(v):
    return np.ascontiguousarray(np.asarray(v, np.float32).reshape(-1, 128).T)


def _p2_inputs(kind, L, I, xT_c, yT_c, memT):
    g = lambda n: np.asarray(I[n])
    pp = np.zeros((128, 112), np.float32)
    for i, n in enumerate(["ln1_g", "ln1_b", "ln2_g", "ln2_b", "ln3_g", "ln3_b"]):
        pp[:, 16 * i:16 * i + 16] = _chunkpack(g(n)[L])
    if kind == "ab":
        pp[:, 96:112] = _chunkpack(g("ssd_norm_w")[0])
    else:
        pp[:, 96:104] = _chunkpack(g("s5_glu_b")[0])
    cst = np.concatenate([np.ones((128, 128), np.float32), np.eye(128, dtype=np.float32)], 1)
    d = {
        "xT": xT_c, "yT": yT_c, "memT": memT,
        "w_out": g("ab_w_out")[0] if kind == "ab" else g("cd_w_out")[0],
        "w_q": g("xa_w_q")[L], "w_kv": g("xa_w_kv")[L], "w_o": g("xa_w_o")[L],
        "w_r": np.ascontiguousarray(np.concatenate([g("moe_w_group")[L], g("moe_w_expert")[L]], 1)),
        "b_r": np.ascontiguousarray(np.concatenate([g("moe_b_group")[L], g("moe_b_expert")[L]])[None, :]),
        "w_gate": g("moe_w_gate")[L], "w_up": g("moe_w_up")[L], "w_down": g("moe_w_down")[L],
        "pp": pp, "cst": cst,
    }
    if kind == "cd":
        d["glu_w"] = g("s5_glu_w")[0]
    return d


def _get(name, fn):
    if name not in _CACHE:
        _CACHE[name] = fn()
    return _CACHE[name]


def _run_p2(kind, L, I, xT, yT, mem):
    nc = _get("p2" + kind, lambda: build_p2(kind))
    maps = []
    memTs = [np.ascontiguousarray(np.asarray(mem[b], np.float32).T) for b in range(2)]
    for c in range(8):
        sl = slice(c * 1024, (c + 1) * 1024)
        maps.append(_p2_inputs(kind, L, I, np.ascontiguousarray(xT[:, sl]), np.ascontiguousarray(yT[:, sl]),
                               memTs[c // 4]))
    res = run_bass_kernel_spmd(nc, maps, core_ids=list(range(8)))
    return np.concatenate([r["xo"] for r in res.results], axis=1)


def kernel(**I):
    g = lambda n: np.asarray(I[n], np.float32)
    x = g("x")
    mem = g("mem")
    xT = np.ascontiguousarray(x.reshape(8192, 2048).T)
    nc = _get("p1ab", build_p1ab)
    maps = []
    for c in range(8):
        d = p1ab_host(g, c)
        d["xT"] = xT
        maps.append(d)
    res = run_bass_kernel_spmd(nc, maps, core_ids=list(range(8)))
    yT = np.empty((3072, 8192), np.float32)
    for c in range(8):
        yT[c * 128:(c + 1) * 128] = res.results[c]["ya"]
        yT[1024 + c * 256:1024 + (c + 1) * 256] = res.results[c]["yb"].reshape(256, 8192)
    x1T = _run_p2("ab", 0, I, xT, yT, mem)
    nc = _get("p1cd", build_p1cd)
    maps = []
    for c in range(8):
        d = p1cd_host(g, c)
        d["xT"] = x1T
        maps.append(d)
    res = run_bass_kernel_spmd(nc, maps, core_ids=list(range(8)))
    yT = np.empty((2048, 8192), np.float32)
    for c in range(8):
        yT[c * 128:(c + 1) * 128] = res.results[c]["gs"]
        yT[1024 + c * 128:1024 + (c + 1) * 128] = res.results[c]["yd"]
    x2T = _run_p2("cd", 1, I, x1T, yT, mem)
    return np.ascontiguousarray(x2T.T).reshape(2, 4096, 2048).astype(np.float32)
```

```python
import contextlib
import numpy as np
import concourse.bass as bass
import concourse.mybir as mybir

F32 = mybir.dt.float32
F32R = mybir.dt.float32r
AF = mybir.ActivationFunctionType
ALU = mybir.AluOpType
AX = mybir.AxisListType

COMPUTE = ("pe", "act", "dve", "pool")
DMAQ = ("sp", "qact", "qpool")
ALPHA = 4.0 ** 0.25
LN_EPS = 1e-5
RMS_EPS = 1e-6


class Prog:
    def __init__(self, nc, ring=8, same_engine_sync=True):
        self.nc = nc
        self.ops = []
        self.cnt = {e: 0 for e in COMPUTE}
        self.ring = ring
        self.dma_i = {q: 0 for q in DMAQ}
        self.dma_val = {}
        self.last_w = {}
        self.readers = {}
        self.waited = {}
        self.same = same_engine_sync
        self.semkeys = set()

    @staticmethod
    def stream_of(eng):
        return {"pe": "pe", "act": "act", "dve": "dve", "pool": "pool",
                "sp": "sp", "qact": "act", "qpool": "pool", "cc": "pool"}[eng]

    def _need(self, stream, dep, waits):
        if dep is None:
            return
        semkey, val, dep_stream, is_dma = dep
        if (not is_dma) and dep_stream == stream and (not self.same or stream == "pe"):
            return
        k = (stream, semkey)
        if self.waited.get(k, 0) >= val:
            return
        self.waited[k] = val
        waits.append((semkey, val))

    def op(self, eng, fn, reads=(), writes=()):
        stream = self.stream_of(eng)
        waits = []
        for r in reads:
            self._need(stream, self.last_w.get(r), waits)
        for w in writes:
            self._need(stream, self.last_w.get(w), waits)
            for d in self.readers.get(w, ()):
                self._need(stream, d, waits)
        if eng == "cc":
            self.cc_cnt = getattr(self, "cc_cnt", 0) + 1
            semkey = ("cc",)
            val = self.cc_cnt
            inc = (semkey, None)
            dep = (semkey, val, stream, True)
            self.dma_val[semkey] = val
        elif eng in COMPUTE:
            self.cnt[eng] += 1
            semkey = ("c", eng)
            val = self.cnt[eng]
            inc = (semkey, 1)
            dep = (semkey, val, stream, False)
        else:
            i = self.dma_i[eng]
            self.dma_i[eng] += 1
            k = i % self.ring
            semkey = ("d", eng, k)
            prev = self.dma_val.get(semkey, 0)
            if prev > 0:
                self._need(stream, (semkey, prev, stream, True), waits)
            val = prev + 16
            self.dma_val[semkey] = val
            inc = (semkey, 16)
            dep = (semkey, val, stream, True)
        self.semkeys.add(semkey)
        for r in reads:
            self.readers.setdefault(r, []).append(dep)
        for w in writes:
            self.last_w[w] = dep
            self.readers[w] = []
        self.ops.append((eng, stream, fn, waits, inc))
        return dep

    def final_waits(self, stream="sp"):
        waits = []
        for semkey, val in list(self.dma_val.items()):
            self._need(stream, (semkey, val, None, True), waits)
        for e in COMPUTE:
            if self.cnt[e]:
                self._need(stream, (("c", e), self.cnt[e], None, True), waits)
        self.ops.append((stream, stream, None, waits, None))

    def emit(self):
        nc = self.nc
        with contextlib.ExitStack() as es:
            sems = {}
            for sk in sorted(self.semkeys, key=str):
                sems[sk] = es.enter_context(nc.semaphore("s_" + "_".join(map(str, sk))))
            block = es.enter_context(nc.Block())
            streams = {"pe": [], "act": [], "dve": [], "pool": [], "sp": []}
            for o in self.ops:
                streams[o[1]].append(o)

            def run(engobj, lst):
                for (eng, stream, fn, waits, inc) in lst:
                    for (sk, val) in waits:
                        engobj.wait_ge(sems[sk], val)
                    if fn is not None:
                        ins = fn(engobj)
                        if inc[1] is None:
                            ins.then_inc(sems[inc[0]])
                        else:
                            ins.then_inc(sems[inc[0]], inc[1])

            @block.tensor
            def _(e):
                run(e, streams["pe"])

            @block.scalar
            def _(e):
                run(e, streams["act"])

            @block.vector
            def _(e):
                run(e, streams["dve"])

            @block.gpsimd
            def _(e):
                run(e, streams["pool"])

            @block.sync
            def _(e):
                run(e, streams["sp"])


class K:
    def __init__(self, P):
        self.P = P

    def dma(self, q, out, in_, reads=(), writes=()):
        self.P.op(q, lambda e: e.dma_start(out=out, in_=in_), reads, writes)

    def mm(self, out, lhsT, rhs, start, stop, reads=(), writes=()):
        self.P.op("pe", lambda e: e.matmul(out, lhsT, rhs, start=start, stop=stop), reads, writes)

    def tr(self, out, in_, ident, reads=(), writes=()):
        self.P.op("pe", lambda e: e.transpose(out, in_, ident), reads, writes)

    def act(self, out, in_, func, reads=(), writes=(), bias=None, scale=None, accum_out=None):
        kw = {}
        if bias is not None:
            kw["bias"] = bias
        if scale is not None:
            kw["scale"] = scale
        if accum_out is not None:
            kw["accum_out"] = accum_out
        self.P.op("act", lambda e: e.activation(out=out, in_=in_, func=func, **kw), reads, writes)

    def tt(self, eng, out, in0, in1, op, reads=(), writes=()):
        self.P.op(eng, lambda e: e.tensor_tensor(out=out, in0=in0, in1=in1, op=op), reads, writes)

    def ts(self, eng, out, in0, s1, s2, op0, op1=None, reads=(), writes=()):
        if op1 is None:
            self.P.op(eng, lambda e: e.tensor_scalar(out=out, in0=in0, scalar1=s1, scalar2=None, op0=op0), reads, writes)
        else:
            self.P.op(eng, lambda e: e.tensor_scalar(out=out, in0=in0, scalar1=s1, scalar2=s2, op0=op0, op1=op1), reads, writes)

    def stt(self, eng, out, in0, scalar, in1, op0, op1, reads=(), writes=()):
        self.P.op(eng, lambda e: e.scalar_tensor_tensor(out=out, in0=in0, scalar=scalar, in1=in1, op0=op0, op1=op1), reads, writes)

    def copy(self, eng, out, in_, reads=(), writes=()):
        if eng == "act":
            self.P.op("act", lambda e: e.activation(out=out, in_=in_, func=AF.Copy), reads, writes)
        else:
            self.P.op(eng, lambda e: e.tensor_copy(out=out, in_=in_), reads, writes)

    def memset(self, eng, out, val, writes=()):
        self.P.op(eng, lambda e: e.memset(out, val), (), writes)

    def scan(self, out, d0, d1, init, op0, op1, reads=(), writes=()):
        self.P.op("dve", lambda e: e.tensor_tensor_scan(out, d0, d1, init, op0, op1), reads, writes)

    def reduce(self, eng, out, in_, op, axis=AX.X, reads=(), writes=()):
        self.P.op(eng, lambda e: e.tensor_reduce(out=out, in_=in_, axis=axis, op=op), reads, writes)


def R(ap):
    return ap.bitcast(F32R)


GK = 1.5957691216057308


def gelu_ops(k, out, x, tmp, rx, rout, rtmp, n=128):
    k.act(tmp, x, AF.Square, reads=[rx], writes=[rtmp])
    k.ts("dve", tmp, tmp, 0.044715, 1.0, ALU.mult, ALU.add, reads=[rtmp], writes=[rtmp])
    k.tt("dve", tmp, tmp, x, ALU.mult, reads=[rtmp, rx], writes=[rtmp])
    k.act(tmp, tmp, AF.Sigmoid, scale=GK, reads=[rtmp], writes=[rtmp])
    k.tt("pool", out, tmp, x, ALU.mult, reads=[rtmp, rx], writes=[rout])


def conv_ops(k, acc, ext, pq, c0, rext, racc, eng="dve"):
    k.ts(eng, acc, ext[:, 0:512], pq[:, c0:c0 + 1], None, ALU.mult, reads=[rext, "pq"], writes=[racc])
    for i in range(1, 4):
        k.stt(eng, acc, ext[:, i:i + 512], pq[:, c0 + i:c0 + i + 1], acc, ALU.mult, ALU.add,
              reads=[rext, "pq", racc], writes=[racc])


def build_p1ab(nbatch=2, nkb=8):
    nc = bass.Bass("TRN2", target_bir_lowering=False)

    def D(name, shape, kind="ExternalInput"):
        return nc.dram_tensor(name, shape, F32, kind=kind).ap()

    xT = D("xT", [2048, 8192])
    w1d = D("w1", [2048, 1028])
    rgwd = D("rgw", [128, 256])
    pqd = D("pq", [128, 48])
    cstd = D("cst", [128, 1408])
    ya_o = D("ya", [128, 8192], "ExternalOutput")
    yb_o = D("yb", [4, 64, 8192], "ExternalOutput")

    with contextlib.ExitStack() as es:
        def S(name, shape):
            return es.enter_context(nc.sbuf_tensor(name + "_s", shape, F32))

        w1 = S("w1", [128, 16 * 1028])
        xb = [S("xb0", [128, 16 * 512])] * 2
        rgw = S("rgw", [128, 256])
        pq = S("pq", [128, 48])
        cst = S("cst", [128, 1408])
        xa_ext = S("xa_ext", [128, 515])
        B_ext = S("B_ext", [128, 515])
        C_ext = S("C_ext", [128, 515])
        xs_ext = S("xs_ext", [64, 4 * 515])
        xg = S("xg", [128, 512])
        ga = S("ga", [128, 512])
        gt = S("gt", [128, 512])
        xc = S("xc", [128, 512])
        r_sb = S("r_sb", [128, 512])
        i_sb = S("i_sb", [128, 512])
        a_sb = S("a_sb", [128, 512])
        u_sb = S("u_sb", [128, 512])
        h_sb = S("h_sb", [128, 512])
        ya_sb = S("ya_sb", [128, 512])
        hprev = S("hprev", [128, 1])
        zs = S("zs", [64, 4 * 512])
        xs_fm = S("xs_fm", [64, 4 * 512])
        xsD = S("xsD", [64, 4 * 512])
        acc = S("acc", [128, 512])
        B_fm = S("B_fm", [128, 512])
        C_fm = S("C_fm", [128, 512])
        dt_fm = S("dt_fm", [4, 512])
        adt = S("adt", [4, 512])
        cs_fm = S("cs_fm", [4, 512])
        sm = S("smab", [128, 16])
        dtcs = S("dtcs", [64, 8])
        Xdt = S("Xdt", [64, 256])
        ecsb = S("ecsb", [128, 256])
        dk = S("dk", [64, 4])
        Bdec = S("Bdec", [64, 4 * 128])
        df = S("df", [64, 256])
        MT = S("MT", [64, 256])
        Cdec = S("Cdec", [128, 256])
        yv = S("yv", [64, 256])
        yb_blk = S("yb_blk", [64, 4 * 512])
        S_sb = S("S_sb", [128, 256])
        ps = [es.enter_context(nc.psum_tensor(f"ps{i}", [128, 512], F32)) for i in range(8)]

        ones = cst[:, 0:128]
        ident = cst[:, 128:256]
        mask = cst[0:64, 256:320]
        sel4 = cst[0:4, 320:832]
        rmask = cst[0:4, 832:1344]
        onecol = cst[:, 0:1]
        w13 = w1[:].rearrange("p (c n) -> p c n", n=1028)
        xs_ext3 = xs_ext[:].rearrange("p (h t) -> p h t", t=515)
        zs3 = zs[:].rearrange("p (h t) -> p h t", t=512)
        xs3 = xs_fm[:].rearrange("p (h t) -> p h t", t=512)
        xsD3 = xsD[:].rearrange("p (h t) -> p h t", t=512)
        yb3 = yb_blk[:].rearrange("p (h t) -> p h t", t=512)
        Xdt3 = Xdt[:].rearrange("p (h q) -> p h q", q=64)
        ecsb3 = ecsb[:].rearrange("p (h l) -> p h l", l=64)
        Bdec3 = Bdec[:].rearrange("p (h n) -> p h n", n=128)
        df3 = df[:].rearrange("p (h l) -> p h l", l=64)
        MT3 = MT[:].rearrange("p (h l) -> p h l", l=64)
        Cdec3 = Cdec[:].rearrange("p (h l) -> p h l", l=64)
        yv3 = yv[:].rearrange("p (h l) -> p h l", l=64)
        S3 = S_sb[:].rearrange("p (h q) -> p h q", q=64)

        import os
        P = Prog(nc, same_engine_sync=os.environ.get("SAME", "1") == "1")
        k = K(P)
        for q4 in range(4):
            k.dma("qpool", R(w13[:, 4 * q4:4 * q4 + 4, :]),
                  w1d[q4 * 512:(q4 + 1) * 512, :].rearrange("(c p) n -> p c n", p=128), writes=["w1"])
        k.dma("qpool", R(rgw[:]), rgwd[:, :], writes=["rgw"])
        k.dma("sp", pq[:], pqd[:, :], writes=["pq"])
        k.dma("sp", cst[:], cstd[:, :], writes=["cst"])
        m8sp = sm[:, 0:1]
        Aneg = sm[0:4, 1:2]
        k.act(m8sp, pq[:, 7:8], AF.Exp, scale=-1.0, reads=["pq"], writes=["sm0"])
        k.act(m8sp, m8sp, AF.Ln, bias=onecol, reads=["sm0", "cst"], writes=["sm0"])
        k.ts("dve", m8sp, m8sp, -8.0, None, ALU.mult, reads=["sm0"], writes=["sm0"])
        k.act(Aneg, pq[0:4, 43:44], AF.Exp, reads=["pq"], writes=["sm1"])
        k.ts("dve", Aneg, Aneg, -1.0, None, ALU.mult, reads=["sm1"], writes=["sm1"])

        for b in range(nbatch):
            k.memset("dve", xa_ext[:, 0:3], 0.0, writes=["xa_ext"])
            k.memset("dve", B_ext[:, 0:3], 0.0, writes=["B_ext"])
            k.memset("dve", C_ext[:, 0:3], 0.0, writes=["C_ext"])
            k.memset("dve", xs_ext[:], 0.0, writes=["xs_ext"])
            k.memset("dve", hprev[:], 0.0, writes=["hprev"])
            k.ts("dve", R(S_sb[:]), cst[:, 0:256], 0.0, None, ALU.mult, reads=["cst"], writes=["S"])
            for kb in range(nkb):
                tok0 = b * 4096 + kb * 512
                bi = (b * nkb + kb) % 2
                xbv = xb[bi][:].rearrange("p (c t) -> p c t", t=512)
                rxb = "xb0"
                for q4 in range(4):
                    k.dma("qpool", R(xbv[:, 4 * q4:4 * q4 + 4, :]),
                          xT[q4 * 512:(q4 + 1) * 512, tok0:tok0 + 512].rearrange("(c p) t -> p c t", p=128),
                          writes=[rxb])

                pcount = [0]

                def proj(col0, M):
                    pn = pcount[0] % 4
                    pcount[0] += 1
                    for kc in range(16):
                        k.mm(ps[pn][0:M, :], R(w13[:, kc, col0:col0 + M]), R(xbv[:, kc, :]),
                             kc == 0, kc == 15, reads=["w1", rxb], writes=[f"ps{pn}"])
                    return ps[pn], f"ps{pn}"

                pt, pr = proj(0, 128)
                k.copy("act", xg[:], pt[:], reads=[pr], writes=["xg"])
                gelu_ops(k, ga[:], xg[:], gt[:], "xg", "ga", "gt")
                pt, pr = proj(128, 128)
                k.copy("act", xa_ext[:, 3:515], pt[:], reads=[pr], writes=["xa_ext"])
                for h in range(4):
                    pt, pr = proj(256 + 64 * h, 64)
                    k.act(zs3[:, h, :], pt[0:64, :], AF.Silu, reads=[pr], writes=["zs"])
                for h in range(4):
                    pt, pr = proj(512 + 64 * h, 64)
                    k.copy("act", xs_ext3[:, h, 3:515], pt[0:64, :], reads=[pr], writes=["xs_ext"])
                pt, pr = proj(768, 128)
                k.copy("act", B_ext[:, 3:515], pt[:], reads=[pr], writes=["B_ext"])
                pt, pr = proj(896, 128)
                k.copy("dve", C_ext[:, 3:515], pt[:], reads=[pr], writes=["C_ext"])
                pt, pr = proj(1024, 4)
                k.act(dt_fm[:], pt[0:4, :], AF.Exp, bias=pq[0:4, 42:43], reads=[pr, "pq"], writes=["dt_fm"])
                k.act(dt_fm[:], dt_fm[:], AF.Ln, bias=onecol[0:4, :], reads=["dt_fm", "cst"], writes=["dt_fm"])
                k.ts("dve", adt[:], dt_fm[:], Aneg, None, ALU.mult, reads=["dt_fm", "sm1"], writes=["adt"])
                k.scan(cs_fm[:], rmask, adt[:], 0.0, ALU.mult, ALU.add, reads=["adt", "cst"], writes=["cs_fm"])

                conv_ops(k, acc[:], xa_ext[:], pq, 0, "xa_ext", "acc")
                k.act(R(xc[:]), acc[:], AF.Identity, bias=pq[:, 4:5], reads=["acc", "pq"], writes=["xc"])
                k.copy("pool", xa_ext[:, 0:3], xa_ext[:, 512:515], reads=["xa_ext"], writes=["xa_ext"])
                k.mm(ps[4][:], R(rgw[:, 0:128]), R(xc[:]), True, True, reads=["rgw", "xc"], writes=["ps4"])
                k.act(r_sb[:], ps[4][:], AF.Sigmoid, bias=pq[:, 5:6], reads=["ps4", "pq"], writes=["r_sb"])
                k.mm(ps[5][:], R(rgw[:, 128:256]), R(xc[:]), True, True, reads=["rgw", "xc"], writes=["ps5"])
                k.act(i_sb[:], ps[5][:], AF.Sigmoid, bias=pq[:, 6:7], reads=["ps5", "pq"], writes=["i_sb"])
                k.act(a_sb[:], r_sb[:], AF.Exp, scale=m8sp, reads=["r_sb", "sm0"], writes=["a_sb"])
                k.tt("pool", r_sb[:], a_sb[:], a_sb[:], ALU.mult, reads=["a_sb"], writes=["r_sb"])
                k.ts("pool", r_sb[:], r_sb[:], 1.0, None, ALU.min, reads=["r_sb"], writes=["r_sb"])
                k.act(r_sb[:], r_sb[:], AF.Sqrt, scale=-1.0, bias=onecol, reads=["r_sb", "cst"], writes=["r_sb"])
                k.tt("dve", u_sb[:], i_sb[:], xc[:], ALU.mult, reads=["i_sb", "xc"], writes=["u_sb"])
                k.tt("dve", u_sb[:], u_sb[:], r_sb[:], ALU.mult, reads=["u_sb", "r_sb"], writes=["u_sb"])
                k.scan(h_sb[:], a_sb[:], u_sb[:], hprev[:, 0:1], ALU.mult, ALU.add,
                       reads=["a_sb", "u_sb", "hprev"], writes=["h_sb"])
                k.copy("pool", hprev[:], h_sb[:, 511:512], reads=["h_sb"], writes=["hprev"])
                k.tt("dve", ya_sb[:], ga[:], h_sb[:], ALU.mult, reads=["ga", "h_sb"], writes=["ya_sb"])
                k.dma("sp", ya_o[:, tok0:tok0 + 512], ya_sb[:], reads=["ya_sb"], writes=[("yao", tok0)])

                for h in range(4):
                    conv_ops(k, acc[0:64, :], xs_ext3[:, h, :], pq[0:64, :], 8 + 5 * h, "xs_ext", "acc",
                             eng="dve")
                    k.act(xs3[:, h, :], acc[0:64, :], AF.Silu, bias=pq[0:64, 12 + 5 * h:13 + 5 * h],
                          reads=["acc", "pq"], writes=["xs_fm"])
                    k.ts("pool", xsD3[:, h, :], xs3[:, h, :], pq[0:64, 38 + h:39 + h], None, ALU.mult,
                         reads=["xs_fm", "pq"], writes=["xsD"])
                k.copy("pool", xs_ext3[:, :, 0:3], xs_ext3[:, :, 512:515], reads=["xs_ext"], writes=["xs_ext"])
                conv_ops(k, acc[:], B_ext[:], pq, 28, "B_ext", "acc")
                k.act(R(B_fm[:]), acc[:], AF.Silu, bias=pq[:, 32:33], reads=["acc", "pq"], writes=["B_fm"])
                k.copy("pool", B_ext[:, 0:3], B_ext[:, 512:515], reads=["B_ext"], writes=["B_ext"])
                conv_ops(k, acc[:], C_ext[:], pq, 33, "C_ext", "acc")
                k.act(R(C_fm[:]), acc[:], AF.Silu, bias=pq[:, 37:38], reads=["acc", "pq"], writes=["C_fm"])
                k.copy("pool", C_ext[:, 0:3], C_ext[:, 512:515], reads=["C_ext"], writes=["C_ext"])

                for ch in range(8):
                    l0 = ch * 64
                    ls = slice(l0, l0 + 64)
                    k.tr(ps[4][0:64, 256:260], dt_fm[0:4, ls], ident[0:4, 0:4], reads=["dt_fm", "cst"], writes=["ps4"])
                    k.tr(ps[4][0:64, 260:264], cs_fm[0:4, ls], ident[0:4, 0:4], reads=["cs_fm", "cst"], writes=["ps4"])
                    k.copy("act", dtcs[:], ps[4][0:64, 256:264], reads=["ps4"], writes=["dtcs"])
                    for h in range(4):
                        k.tr(ps[4][0:64, h * 64:(h + 1) * 64], xs3[:, h, ls], ident[0:64, 0:64],
                             reads=["xs_fm", "cst"], writes=["ps4"])
                    k.tt("dve", R(Xdt3), ps[4][0:64, 0:256].rearrange("p (h q) -> p h q", q=64),
                         dtcs[:, 0:4].unsqueeze(2).to_broadcast([64, 4, 64]), ALU.mult,
                         reads=["ps4", "dtcs"], writes=["Xdt"])
                    for h in range(4):
                        k.mm(ps[5][:, h * 64:(h + 1) * 64], sel4[:, h * 128:(h + 1) * 128], cs_fm[0:4, ls],
                             True, True, reads=["cst", "cs_fm"], writes=["ps5"])
                    psc3 = ps[5][:, 0:256].rearrange("p (h l) -> p h l", l=64)
                    k.act(ecsb[:], ps[5][:, 0:256], AF.Exp, reads=["ps5"], writes=["ecsb"])
                    k.tt("dve", dk[:], psc3[0:64, :, 63], dtcs[:, 4:8], ALU.subtract,
                         reads=["ps5", "dtcs"], writes=["dk"])
                    k.act(dk[:], dk[:], AF.Exp, reads=["dk"], writes=["dk"])
                    k.tr(ps[6][0:64, 0:128], B_fm[:, ls], ident, reads=["B_fm", "cst"], writes=["ps6"])
                    for h in range(4):
                        if h % 2 == 0:
                            k.ts("dve", R(Bdec3[:, h, :]), ps[6][0:64, 0:128], dk[:, h:h + 1], None, ALU.mult,
                                 reads=["ps6", "dk"], writes=["Bdec"])
                        else:
                            k.act(R(Bdec3[:, h, :]), ps[6][0:64, 0:128], AF.Copy, scale=dk[:, h:h + 1],
                                  reads=["ps6", "dk"], writes=["Bdec"])
                    k.mm(ps[5][0:64, 256:320], R(B_fm[:, ls]), R(C_fm[:, ls]), True, True,
                         reads=["B_fm", "C_fm"], writes=["ps5"])
                    k.tt("dve", df3, psc3[0:64], dtcs[:, 4:8].unsqueeze(2).to_broadcast([64, 4, 64]), ALU.subtract,
                         reads=["ps5", "dtcs"], writes=["df"])
                    k.ts("dve", df[:], df[:], 0.0, None, ALU.min, reads=["df"], writes=["df"])
                    k.act(df[:], df[:], AF.Exp, reads=["df"], writes=["df"])
                    k.tt("pool", df3, df3, mask.unsqueeze(1).to_broadcast([64, 4, 64]), ALU.mult,
                         reads=["df", "cst"], writes=["df"])
                    k.tt("dve", R(MT3), df3, ps[5][0:64, 256:320].unsqueeze(1).to_broadcast([64, 4, 64]), ALU.mult,
                         reads=["df", "ps5"], writes=["MT"])
                    k.tt("pool", R(Cdec3), ecsb3, C_fm[:, ls].unsqueeze(1).to_broadcast([128, 4, 64]), ALU.mult,
                         reads=["ecsb", "C_fm"], writes=["Cdec"])
                    for h in range(4):
                        k.mm(ps[6][0:64, 128 + h * 64:128 + (h + 1) * 64], R(Xdt3[:, h, :]), R(MT3[:, h, :]),
                             True, False, reads=["Xdt", "MT"], writes=["ps6"])
                        k.mm(ps[6][0:64, 128 + h * 64:128 + (h + 1) * 64], R(S3[:, h, :]), R(Cdec3[:, h, :]),
                             False, True, reads=["S", "Cdec"], writes=["ps6"])
                    k.tt("dve", yv3, ps[6][0:64, 128:384].rearrange("p (h l) -> p h l", l=64), xsD3[:, :, ls],
                         ALU.add, reads=["ps6", "xsD"], writes=["yv"])
                    k.tt("pool", yb3[:, :, ls], yv3, zs3[:, :, ls], ALU.mult, reads=["yv", "zs"], writes=["yb_blk"])
                    for h in range(4):
                        k.mm(ps[7][:, h * 64:(h + 1) * 64], R(Bdec3[:, h, :]), R(Xdt3[:, h, :]), True, True,
                             reads=["Bdec", "Xdt"], writes=["ps7"])
                    for h in range(4):
                        k.stt("dve", R(S3[:, h, :]), S3[:, h, :], ecsb3[:, h, 63:64], ps[7][:, h * 64:(h + 1) * 64],
                              ALU.mult, ALU.add, reads=["S", "ecsb", "ps7"], writes=["S"])
                k.dma("sp", yb_o[:, :, tok0:tok0 + 512].rearrange("h p t -> p h t"), yb3,
                      reads=["yb_blk"], writes=[("ybo", tok0)])
        P.final_waits("sp")
        P.emit()
    return nc


def p1ab_host(g, c):
    W = g("ab_w_in")[0]
    grp = c // 4
    cols = [np.arange(c * 128, (c + 1) * 128), 1024 + np.arange(c * 128, (c + 1) * 128)]
    zb = 2048
    cols.append(zb + np.arange(256 * c, 256 * (c + 1)))
    xb0 = 2048 + 2048
    cols.append(xb0 + np.arange(256 * c, 256 * (c + 1)))
    cols.append(xb0 + 2048 + np.arange(128 * grp, 128 * (grp + 1)))
    cols.append(xb0 + 2048 + 256 + np.arange(128 * grp, 128 * (grp + 1)))
    cols.append(xb0 + 2560 + np.arange(4 * c, 4 * c + 4))
    cols = np.concatenate(cols)
    w1 = np.ascontiguousarray(W[:, cols])
    rgw = np.ascontiguousarray(np.concatenate([g("rg_wa")[0][c], g("rg_wx")[0][c]], 1))
    pq = np.zeros((128, 48), np.float32)
    sl = slice(c * 128, (c + 1) * 128)
    pq[:, 0:4] = g("rg_conv_w")[0][:, sl].T
    pq[:, 4] = g("rg_conv_b")[0][sl]
    pq[:, 5] = g("rg_ba")[0][sl]
    pq[:, 6] = g("rg_bx")[0][sl]
    pq[:, 7] = g("rg_lam")[0][sl]
    cw = g("ssd_conv_w")[0]
    cb = g("ssd_conv_b")[0]
    for h in range(4):
        hs = slice(256 * c + 64 * h, 256 * c + 64 * (h + 1))
        pq[0:64, 8 + 5 * h:12 + 5 * h] = cw[:, hs].T
        pq[0:64, 12 + 5 * h] = cb[hs]
        pq[0:64, 38 + h] = g("ssd_d")[0][4 * c + h]
    bs = slice(2048 + 128 * grp, 2048 + 128 * (grp + 1))
    cs_ = slice(2048 + 256 + 128 * grp, 2048 + 256 + 128 * (grp + 1))
    pq[:, 28:32] = cw[:, bs].T
    pq[:, 32] = cb[bs]
    pq[:, 33:37] = cw[:, cs_].T
    pq[:, 37] = cb[cs_]
    pq[0:4, 42] = g("ssd_dt_bias")[0][4 * c:4 * c + 4]
    pq[0:4, 43] = g("ssd_a_log")[0][4 * c:4 * c + 4]
    return {"w1": w1, "rgw": rgw, "pq": pq, "cst": ab_cst()}


def ab_cst():
    cst = np.zeros((128, 1408), np.float32)
    cst[:, 0:128] = 1.0
    cst[:, 128:256] = np.eye(128, dtype=np.float32)
    s = np.arange(64)[:, None]
    l = np.arange(64)[None, :]
    cst[0:64, 256:320] = (s <= l).astype(np.float32)
    for h in range(4):
        cst[h, 320 + h * 128:320 + (h + 1) * 128] = 1.0
    rm = np.ones(512, np.float32)
    rm[::64] = 0.0
    cst[0:4, 832:1344] = rm[None, :]
    return cst

import math

TWO_PI = 2.0 * math.pi


def build_p1cd(nbatch=2, nkb=8, do_s5=True, do_gdn=True, stage=9):
    nc = bass.Bass("TRN2", target_bir_lowering=False)

    def D(name, shape, kind="ExternalInput"):
        return nc.dram_tensor(name, shape, F32, kind=kind).ap()

    xT = D("xT", [2048, 8192])
    w1d = D("w1", [2048, 642])
    pqd = D("pq", [128, 64])
    cstd = D("cst", [128, 1792])
    lvd = D("lv", [64, 768])
    s5md = D("s5m", [128, 2048])
    nwd = D("nw", [1, 128])
    gs_o = D("gs", [128, 8192], "ExternalOutput")
    yd_o = D("yd", [128, 8192], "ExternalOutput")

    with contextlib.ExitStack() as es:
        def S(name, shape):
            return es.enter_context(nc.sbuf_tensor(name + "_s", shape, F32))

        w1 = S("w1", [128, 16 * 642])
        xb = S("xb", [128, 16 * 512])
        pq = S("pq", [128, 64])
        cst = S("cst", [128, 1792])
        lv = S("lv", [64, 768])
        s5m = S("s5m", [128, 2048])
        nwb = S("nwb", [64, 128])
        sm = S("sm", [128, 64])
        tabs = S("tabs", [128, 16 * 512])
        rhoT = S("rhoT", [128, 4 * 512])
        u_fm = S("u_fm", [128, 512])
        wk = [S(f"wk{i}", [128, 512]) for i in range(8)]
        hr = S("hr", [128, 512])
        hi = S("hi", [128, 512])
        hprev = S("hprev", [128, 8])
        gin = S("gin", [128, 8])
        ys, gt, gout = wk[0], wk[1], wk[2]
        q_ext = S("q_ext", [128, 515])
        k_ext = S("k_ext", [128, 515])
        v_ext = S("v_ext", [128, 515])
        zs = S("zs", [128, 512])
        q_fm = S("q_fm", [128, 512])
        k_fm = S("k_fm", [128, 512])
        v_fm = S("v_fm", [128, 512])
        acc = S("acc", [128, 512])
        g_fm = S("g_fm", [1, 512])
        b_fm = S("b_fm", [1, 512])
        gcs_fm = S("gcs_fm", [1, 512])
        A_all, AT_all, Dm, DmT = (t[0:64, :] for t in (wk[6], wk[7], wk[4], wk[5]))
        Ek = S("Ek", [64, 512])
        EkT = S("EkT", [64, 512])
        X_sb = S("X_sb", [64, 512])
        Z_sb = S("Z_sb", [64, 512])
        qkT_all = S("qkT_all", [64, 512])
        tokc = S("tokc", [64, 8 * 8])
        egcsb = S("egcsb", [128, 512])
        rhs_u = S("rhs_u", [64, 128])
        rhs_w = S("rhs_w", [64, 128])
        kdec = S("kdec", [64, 128])
        u_sb = S("u_sb", [64, 128])
        wT = S("wT", [128, 64])
        qdec = S("qdec", [128, 64])
        vnew = S("vnew", [64, 128])
        o_sb = S("o_sb", [64, 128])
        osq = S("osq", [64, 128])
        S_sb = S("S_sb", [128, 128])
        sqn = hr
        onesR = S("onesR", [128, 128])
        Dd = S("Dd", [64, 512])
        DdT = S("DdT", [64, 512])
        yd_blk = S("yd_blk", [128, 512])
        ps = [es.enter_context(nc.psum_tensor(f"ps{i}", [128, 512], F32)) for i in range(8)]
        qi = es.enter_context(nc.sbuf_tensor("qi_s", [128, 512], mybir.dt.int32))

        ones = cst[:, 0:128]
        ident = cst[:, 128:256]
        iota = cst[:, 256:768]
        mask_le = cst[0:64, 768:832]
        mask_lt = cst[0:64, 832:896]
        mask_gt = cst[0:64, 896:960]
        rmask = cst[0:1, 1024:1536]
        onesrow = cst[0:1, 0:128]
        onecol = cst[:, 0:1]
        w13 = w1[:].rearrange("p (c n) -> p c n", n=642)
        xbv = xb[:].rearrange("p (c t) -> p c t", t=512)
        tab3 = tabs[:].rearrange("p (i t) -> p i t", t=512)
        rho3 = rhoT[:].rearrange("p (i t) -> p i t", t=512)
        c3 = lambda t: t[:, :].rearrange("p (c l) -> p c l", l=64)

        import os
        P = Prog(nc, same_engine_sync=os.environ.get("SAME", "1") == "1")
        k = K(P)
        for q4 in range(4):
            k.dma("qpool", R(w13[:, 4 * q4:4 * q4 + 4, :]),
                  w1d[q4 * 512:(q4 + 1) * 512, :].rearrange("(c p) n -> p c n", p=128), writes=["w1"])
        k.dma("sp", pq[:], pqd[:, :], writes=["pq"])
        k.dma("sp", cst[:], cstd[:, :], writes=["cst"])
        k.dma("sp", lv[:], lvd[:, :], writes=["lv"])
        k.dma("qpool", R(s5m[:]), s5md[:, :], writes=["s5m"])
        k.dma("sp", nwb[:], nwd[0].partition_broadcast(64), writes=["nwb"])

        rr = ["sm"]
        step = sm[:, 0:4]
        ars = sm[:, 4:8]
        th = sm[:, 8:12]
        rho = sm[:, 12:16]
        cth = sm[:, 16:20]
        sth = sm[:, 20:24]
        lbr = sm[:, 24:28]
        lbi = sm[:, 28:32]
        den = sm[:, 32:36]
        fre = sm[:, 36:40]
        fim = sm[:, 40:44]
        t4a = sm[:, 44:48]
        t4b = sm[:, 48:52]
        nfre = sm[:, 52:56]
        Aneg = sm[0:1, 56:57]

        def sin_to(out, ang, shift, n, rres, wres):
            qf = wk[3][:, 0:n]
            tmp = wk[2][:, 0:n]
            qiv = qi[:, 0:n]
            rq = ["wk3", "wk2", "qi"]
            k.ts("dve", qf, ang, shift, 1.0 / TWO_PI, ALU.add, ALU.mult, reads=rres, writes=rq)
            k.copy("dve", qiv, qf, reads=rq, writes=rq)
            k.copy("dve", qf, qiv, reads=rq, writes=rq)
            k.ts("dve", out, ang, shift, None, ALU.add, reads=rres, writes=wres)
            k.stt("dve", out, qf, -TWO_PI, out, ALU.mult, ALU.add, reads=rq + wres, writes=wres)
            k.ts("dve", tmp, out, math.pi, -TWO_PI, ALU.is_gt, ALU.mult, reads=wres, writes=rq)
            k.tt("dve", out, out, tmp, ALU.add, reads=rq + wres, writes=wres)
            k.ts("dve", tmp, out, -math.pi, TWO_PI, ALU.is_lt, ALU.mult, reads=wres, writes=rq)
            k.tt("dve", out, out, tmp, ALU.add, reads=rq + wres, writes=wres)
            k.ts("dve", out, out, math.pi, -math.pi, ALU.min, ALU.max, reads=wres, writes=wres)
            k.act(out, out, AF.Sin, reads=wres, writes=wres)

        if do_s5:
            k.act(step, pq[:, 8:12], AF.Exp, reads=["pq"], writes=rr)
            k.tt("dve", ars, pq[:, 0:4], step, ALU.mult, reads=rr + ["pq"], writes=rr)
            k.tt("dve", th, pq[:, 4:8], step, ALU.mult, reads=rr + ["pq"], writes=rr)
            k.act(rho, ars, AF.Exp, reads=rr, writes=rr)
            sin_to(cth, th, math.pi / 2, 4, rr, rr)
            sin_to(sth, th, 0.0, 4, rr, rr)
            k.tt("dve", lbr, rho, cth, ALU.mult, reads=rr, writes=rr)
            k.tt("dve", lbi, rho, sth, ALU.mult, reads=rr, writes=rr)
            k.tt("dve", den, pq[:, 0:4], pq[:, 0:4], ALU.mult, reads=rr + ["pq"], writes=rr)
            k.tt("dve", t4a, pq[:, 4:8], pq[:, 4:8], ALU.mult, reads=rr + ["pq"], writes=rr)
            k.tt("dve", den, den, t4a, ALU.add, reads=rr, writes=rr)
            k.P.op("dve", lambda e: e.reciprocal(den, den), reads=rr, writes=rr)
            k.ts("dve", t4a, lbr, -1.0, None, ALU.add, reads=rr, writes=rr)
            k.tt("dve", fre, t4a, pq[:, 0:4], ALU.mult, reads=rr + ["pq"], writes=rr)
            k.tt("dve", t4b, lbi, pq[:, 4:8], ALU.mult, reads=rr + ["pq"], writes=rr)
            k.tt("dve", fre, fre, t4b, ALU.add, reads=rr, writes=rr)
            k.tt("dve", fre, fre, den, ALU.mult, reads=rr, writes=rr)
            k.tt("dve", fim, lbi, pq[:, 0:4], ALU.mult, reads=rr + ["pq"], writes=rr)
            k.tt("dve", t4b, t4a, pq[:, 4:8], ALU.mult, reads=rr + ["pq"], writes=rr)
            k.tt("dve", fim, fim, t4b, ALU.subtract, reads=rr, writes=rr)
            k.tt("dve", fim, fim, den, ALU.mult, reads=rr, writes=rr)
            k.ts("dve", nfre, fre, -1.0, None, ALU.mult, reads=rr, writes=rr)
            for i in range(4):
                Fr, Fi, Cr, Ci = (tab3[:, 4 * i + j, :] for j in range(4))
                ang = wk[0][:]
                k.ts("dve", ang, iota, th[:, i:i + 1], None, ALU.mult, reads=rr + ["cst"], writes=["wk0"])
                sin_to(Cr, ang, math.pi / 2, 512, ["wk0"], ["tabs"])
                sin_to(Ci, ang, 0.0, 512, ["wk0"], ["tabs"])
                k.ts("dve", Fr, Cr, fre[:, i:i + 1], None, ALU.mult, reads=rr + ["tabs"], writes=["tabs"])
                k.stt("dve", Fr, Ci, fim[:, i:i + 1], Fr, ALU.mult, ALU.add, reads=rr + ["tabs"], writes=["tabs"])
                k.ts("dve", Fi, Cr, fim[:, i:i + 1], None, ALU.mult, reads=rr + ["tabs"], writes=["tabs"])
                k.stt("dve", Fi, Ci, nfre[:, i:i + 1], Fi, ALU.mult, ALU.add, reads=rr + ["tabs"], writes=["tabs"])
                k.act(rho3[:, i, :], iota, AF.Identity, scale=0.0, bias=rho[:, i:i + 1],
                      reads=rr + ["cst"], writes=["rhoT"])
        k.memset("dve", sm[:, 61:62], 1e-6, writes=["eps6"])
        k.ts("dve", R(onesR[:]), cst[:, 0:128], 1.0, None, ALU.mult, reads=["cst"], writes=["onesR"])
        if do_gdn:
            k.act(Aneg, pq[0:1, 28:29], AF.Exp, reads=["pq"], writes=["Aneg"])
            k.ts("dve", Aneg, Aneg, -1.0, None, ALU.mult, reads=["Aneg"], writes=["Aneg"])

        for b in range(nbatch):
            k.memset("dve", hprev[:], 0.0, writes=["hprev"])
            k.memset("dve", q_ext[:, 0:3], 0.0, writes=["q_ext"])
            k.memset("dve", k_ext[:, 0:3], 0.0, writes=["k_ext"])
            k.memset("dve", v_ext[:, 0:3], 0.0, writes=["v_ext"])
            k.ts("dve", R(S_sb[:]), cst[:, 0:128], 0.0, None, ALU.mult, reads=["cst"], writes=["S"])
            for kb in range(nkb):
                tok0 = b * 4096 + kb * 512
                for q4 in range(4):
                    k.dma("qpool", R(xbv[:, 4 * q4:4 * q4 + 4, :]),
                          xT[q4 * 512:(q4 + 1) * 512, tok0:tok0 + 512].rearrange("(c p) t -> p c t", p=128),
                          writes=["xb"])
                pcount = [0]

                def proj(col0, M):
                    pn = pcount[0] % 2
                    pcount[0] += 1
                    for kc in range(16):
                        k.mm(ps[pn][0:M, :], R(w13[:, kc, col0:col0 + M]), R(xbv[:, kc, :]),
                             kc == 0, kc == 15, reads=["w1", "xb"], writes=[f"ps{pn}"])
                    return ps[pn], f"ps{pn}"

                if do_s5:
                    pt, pr = proj(0, 128)
                    k.copy("act", R(u_fm[:]), pt[:], reads=[pr], writes=["u_fm"])
                    for i in range(4):
                        Fr, Fi, Cr, Ci = (tab3[:, 4 * i + j, :] for j in range(4))
                        k.mm(ps[2][:], R(s5m[:, i * 128:(i + 1) * 128]), R(u_fm[:]), True, True,
                             reads=["s5m", "u_fm"], writes=["ps2"])
                        k.mm(ps[3][:], R(s5m[:, 512 + i * 128:512 + (i + 1) * 128]), R(u_fm[:]), True, True,
                             reads=["s5m", "u_fm"], writes=["ps3"])
                        k.tt("dve", wk[0][:], ps[2][:], Fr, ALU.mult, reads=["ps2", "tabs"], writes=["wk0"])
                        k.tt("dve", wk[1][:], ps[3][:], Fi, ALU.mult, reads=["ps3", "tabs"], writes=["wk1"])
                        k.tt("pool", wk[0][:], wk[0][:], wk[1][:], ALU.subtract, reads=["wk0", "wk1"], writes=["wk0"])
                        k.tt("dve", wk[2][:], ps[2][:], Fi, ALU.mult, reads=["ps2", "tabs"], writes=["wk2"])
                        k.tt("dve", wk[3][:], ps[3][:], Fr, ALU.mult, reads=["ps3", "tabs"], writes=["wk3"])
                        k.tt("pool", wk[2][:], wk[2][:], wk[3][:], ALU.add, reads=["wk2", "wk3"], writes=["wk2"])
                        hpr = hprev[:, 2 * i:2 * i + 1]
                        hpi = hprev[:, 2 * i + 1:2 * i + 2]
                        gr0 = gin[:, 2 * i:2 * i + 1]
                        gi0 = gin[:, 2 * i + 1:2 * i + 2]
                        tA = gin[:, 0:1] if False else sm[:, 60:61]
                        rg = ["gin"]
                        k.tt("dve", gr0, hpr, cth[:, i:i + 1], ALU.mult, reads=["hprev", "sm"], writes=rg)
                        k.tt("dve", tA, hpi, sth[:, i:i + 1], ALU.mult, reads=["hprev", "sm"], writes=["tA"])
                        k.tt("dve", gr0, gr0, tA, ALU.subtract, reads=rg + ["tA"], writes=rg)
                        k.tt("dve", gi0, hpi, cth[:, i:i + 1], ALU.mult, reads=["hprev", "sm"], writes=rg)
                        k.tt("dve", tA, hpr, sth[:, i:i + 1], ALU.mult, reads=["hprev", "sm"], writes=["tA"])
                        k.tt("dve", gi0, gi0, tA, ALU.add, reads=rg + ["tA"], writes=rg)
                        k.scan(wk[4][:], rho3[:, i, :], wk[0][:], gr0, ALU.mult, ALU.add,
                               reads=["rhoT", "wk0"] + rg, writes=["wk4"])
                        k.scan(wk[5][:], rho3[:, i, :], wk[2][:], gi0, ALU.mult, ALU.add,
                               reads=["rhoT", "wk2"] + rg, writes=["wk5"])
                        k.tt("pool", wk[6][:], wk[4][:], Cr, ALU.mult, reads=["wk4", "tabs"], writes=["wk6"])
                        k.tt("pool", wk[7][:], wk[5][:], Ci, ALU.mult, reads=["wk5", "tabs"], writes=["wk7"])
                        k.tt("dve", R(hr[:]), wk[6][:], wk[7][:], ALU.subtract, reads=["wk6", "wk7"], writes=["hr"])
                        k.tt("pool", wk[6][:], wk[5][:], Cr, ALU.mult, reads=["wk5", "tabs"], writes=["wk6"])
                        k.tt("pool", wk[7][:], wk[4][:], Ci, ALU.mult, reads=["wk4", "tabs"], writes=["wk7"])
                        k.tt("dve", R(hi[:]), wk[6][:], wk[7][:], ALU.add, reads=["wk6", "wk7"], writes=["hi"])
                        k.copy("pool", hpr, hr[:, 511:512], reads=["hr"], writes=["hprev"])
                        k.copy("pool", hpi, hi[:, 511:512], reads=["hi"], writes=["hprev"])
                        k.mm(ps[4][:], R(s5m[:, 1024 + i * 128:1024 + (i + 1) * 128]), R(hr[:]), i == 0, i == 3,
                             reads=["s5m", "hr"], writes=["ps4"])
                        k.mm(ps[5][:], R(s5m[:, 1536 + i * 128:1536 + (i + 1) * 128]), R(hi[:]), i == 0, i == 3,
                             reads=["s5m", "hi"], writes=["ps5"])
                    k.copy("act", ys[:], ps[5][:], reads=["ps5"], writes=["wk0"])
                    k.tt("dve", ys[:], ps[4][:], ys[:], ALU.subtract, reads=["ps4", "wk0"], writes=["wk0"])
                    k.stt("dve", ys[:], u_fm[:], pq[:, 12:13], ys[:], ALU.mult, ALU.add,
                          reads=["u_fm", "pq", "wk0"], writes=["wk0"])
                    gelu_ops(k, gout[:], ys[:], gt[:], "wk0", "wk2", "wk1")
                    k.dma("sp", gs_o[:, tok0:tok0 + 512], gout[:], reads=["wk2"], writes=[("gso", tok0)])

                if not do_gdn:
                    continue
                for (ext, col, nm) in ((q_ext, 128, "q_ext"), (k_ext, 256, "k_ext"), (v_ext, 384, "v_ext")):
                    pt, pr = proj(col, 128)
                    k.copy("act", ext[:, 3:515], pt[:], reads=[pr], writes=[nm])
                pt, pr = proj(512, 128)
                k.act(zs[:], pt[:], AF.Silu, reads=[pr], writes=["zs"])
                pt, pr = proj(640, 1)
                k.act(g_fm[:], pt[0:1, :], AF.Exp, bias=pq[0:1, 29:30], reads=[pr, "pq"], writes=["g_fm"])
                k.act(g_fm[:], g_fm[:], AF.Ln, bias=onecol[0:1, :], reads=["g_fm", "cst"], writes=["g_fm"])
                k.ts("dve", g_fm[:], g_fm[:], Aneg, None, ALU.mult, reads=["g_fm", "Aneg"], writes=["g_fm"])
                k.scan(gcs_fm[:], rmask, g_fm[:], 0.0, ALU.mult, ALU.add, reads=["g_fm", "cst"], writes=["gcs_fm"])
                pt, pr = proj(641, 1)
                k.act(b_fm[:], pt[0:1, :], AF.Sigmoid, reads=[pr], writes=["b_fm"])
                if stage < 1:
                    continue
                for (ext, fm, c0, nm, fn) in ((q_ext, q_fm, 16, "q_ext", "q_fm"), (k_ext, k_fm, 20, "k_ext", "k_fm"),
                                              (v_ext, v_fm, 24, "v_ext", "v_fm")):
                    conv_ops(k, acc[:], ext[:], pq, c0, nm, "acc")
                    k.act(R(fm[:]), acc[:], AF.Silu, reads=["acc"], writes=[fn])
                    k.copy("pool", ext[:, 0:3], ext[:, 512:515], reads=[nm], writes=[nm])
                for (fm, fn, scl) in ((q_fm, "q_fm", 128.0 ** -0.5), (k_fm, "k_fm", 1.0)):
                    k.act(R(sqn[:]), fm[:], AF.Square, reads=[fn], writes=["hr"])
                    k.mm(ps[2][:], R(onesR[:]), R(sqn[:]), True, True, reads=["hr", "onesR"], writes=["ps2"])
                    k.act(acc[:], ps[2][:], AF.Sqrt, bias=sm[:, 61:62], reads=["ps2", "eps6"], writes=["acc"])
                    k.P.op("dve", lambda e: e.reciprocal(acc[:], acc[:]), reads=["acc"], writes=["acc"])
                    k.stt("dve", R(fm[:]), fm[:], scl, acc[:], ALU.mult, ALU.mult, reads=[fn, "acc"], writes=[fn])
                if stage < 2:
                    continue
                for ch in range(8):
                    ls = slice(ch * 64, ch * 64 + 64)
                    tc_ = tokc[:, 8 * ch:8 * ch + 8]
                    rt = [("tokc", ch)]
                    k.tr(ps[2][0:64, 0:1], gcs_fm[0:1, ls], ident[0:1, 0:1], reads=["gcs_fm", "cst"], writes=["ps2"])
                    k.tr(ps[2][0:64, 1:2], b_fm[0:1, ls], ident[0:1, 0:1], reads=["b_fm", "cst"], writes=["ps2"])
                    k.copy("act", tc_[:, 0:2], ps[2][0:64, 0:2], reads=["ps2"], writes=rt)
                    k.mm(ps[3][:, ls], onesrow, gcs_fm[0:1, ls], True, True, reads=["cst", "gcs_fm"], writes=["ps3"])
                    k.mm(ps[4][0:64, ls], onesrow[:, 0:64], b_fm[0:1, ls], True, True, reads=["cst", "b_fm"], writes=["ps4"])
                    k.ts("dve", Dm[:, ls], ps[3][0:64, ls], tc_[:, 0:1], 0.0, ALU.subtract, ALU.min,
                         reads=["ps3"] + rt, writes=["wk4"])
                    k.ts("dve", DmT[:, ls], ps[3][0:64, ls], tc_[:, 0:1], -1.0, ALU.subtract, ALU.mult,
                         reads=["ps3"] + rt, writes=["wk5"])
                    k.ts("dve", DmT[:, ls], DmT[:, ls], 0.0, None, ALU.min, reads=["wk5"], writes=["wk5"])
                    k.act(tc_[:, 2:3], tc_[:, 0:1], AF.Exp, reads=rt, writes=rt)
                    k.tt("dve", tc_[:, 3:4], ps[3][0:64, ch * 64 + 63:ch * 64 + 64], tc_[:, 0:1], ALU.subtract,
                         reads=["ps3"] + rt, writes=rt)
                    k.act(tc_[:, 3:4], tc_[:, 3:4], AF.Exp, reads=rt, writes=rt)
                    k.tt("dve", tc_[:, 4:5], tc_[:, 1:2], tc_[:, 2:3], ALU.mult, reads=rt, writes=rt)
                    k.mm(ps[5][0:64, ls], R(k_fm[:, ls]), R(k_fm[:, ls]), True, True, reads=["k_fm"], writes=["ps5"])
                    k.mm(ps[6][0:64, ls], R(k_fm[:, ls]), R(q_fm[:, ls]), True, True, reads=["k_fm", "q_fm"], writes=["ps6"])
                if stage < 3:
                    continue
                k.act(egcsb[:], ps[3][:], AF.Exp, reads=["ps3"], writes=["egcsb"])
                k.act(Dm[:], Dm[:], AF.Exp, reads=["wk4"], writes=["wk4"])
                k.act(DmT[:], DmT[:], AF.Exp, reads=["wk5"], writes=["wk5"])
                k.tt("dve", AT_all[:], ps[5][0:64, :], ps[4][0:64, :], ALU.mult, reads=["ps5", "ps4"], writes=["wk7"]) \
                    if False else None
                k.copy("act", acc[0:64, :], ps[4][0:64, :], reads=["ps4"], writes=["acc"])
                k.tt("dve", AT_all[:], ps[5][0:64, :], acc[0:64, :], ALU.mult, reads=["ps5", "acc"], writes=["wk7"])
                k.tt("pool", AT_all[:], AT_all[:], Dm[:], ALU.mult, reads=["wk7", "wk4"], writes=["wk7"])
                k.tt("pool", c3(AT_all), c3(AT_all), mask_lt.unsqueeze(1).to_broadcast([64, 8, 64]), ALU.mult,
                     reads=["wk7", "cst"], writes=["wk7"])
                k.tt("dve", A_all[:], ps[5][0:64, :], DmT[:], ALU.mult, reads=["ps5", "wk5"], writes=["wk6"])
                k.tt("pool", c3(A_all), c3(A_all), mask_gt.unsqueeze(1).to_broadcast([64, 8, 64]), ALU.mult,
                     reads=["wk6", "cst"], writes=["wk6"])
                for ch in range(8):
                    ls = slice(ch * 64, ch * 64 + 64)
                    k.ts("dve", A_all[:, ls], A_all[:, ls], tokc[:, 8 * ch + 1:8 * ch + 2], None, ALU.mult,
                         reads=["wk6", ("tokc", ch)], writes=["wk6"])
                k.tt("dve", R(qkT_all[:]), ps[6][0:64, :], Dm[:], ALU.mult, reads=["ps6", "wk4"], writes=["qkT_all"])
                k.tt("pool", R(c3(qkT_all)), c3(qkT_all), mask_le.unsqueeze(1).to_broadcast([64, 8, 64]), ALU.mult,
                     reads=["qkT_all", "cst"], writes=["qkT_all"])
                if stage < 4:
                    continue
                I8 = ident[0:64, 0:64].unsqueeze(1).to_broadcast([64, 8, 64])
                k.tt("dve", R(c3(Ek)), c3(A_all), lv[:, 0:64].unsqueeze(1).to_broadcast([64, 8, 64]), ALU.mult,
                     reads=["wk6", "lv"], writes=["Ek"])
                k.tt("dve", R(c3(Dd)), I8, c3(Ek), ALU.subtract, reads=["Ek", "cst"], writes=["Dd"])
                k.tt("pool", R(c3(EkT)), c3(AT_all), lv[:, 384:448].unsqueeze(1).to_broadcast([64, 8, 64]), ALU.mult,
                     reads=["wk7", "lv"], writes=["EkT"])
                k.tt("pool", R(c3(DdT)), I8, c3(EkT), ALU.subtract, reads=["EkT", "cst"], writes=["DdT"])
                for lev in range(1, 6):
                    k.tt("dve", R(c3(Ek)), c3(A_all), lv[:, 64 * lev:64 * lev + 64].unsqueeze(1).to_broadcast([64, 8, 64]),
                         ALU.mult, reads=["wk6", "lv"], writes=["Ek"])
                    k.tt("pool", R(c3(EkT)), c3(AT_all),
                         lv[:, 384 + 64 * lev:384 + 64 * lev + 64].unsqueeze(1).to_broadcast([64, 8, 64]),
                         ALU.mult, reads=["wk7", "lv"], writes=["EkT"])
                    for ch in range(8):
                        ls = slice(ch * 64, ch * 64 + 64)
                        k.mm(ps[2][0:64, ls], R(EkT[:, ls]), R(Dd[:, ls]), True, True, reads=["EkT", "Dd"], writes=["ps2"])
                    k.copy("act", R(X_sb[:]), ps[2][0:64, :], reads=["ps2"], writes=["X_sb"])
                    for ch in range(8):
                        ls = slice(ch * 64, ch * 64 + 64)
                        k.mm(ps[4][0:64, ls], R(Ek[:, ls]), R(DdT[:, ls]), True, True, reads=["Ek", "DdT"], writes=["ps4"])
                    k.copy("act", R(Z_sb[:]), ps[4][0:64, :], reads=["ps4"], writes=["Z_sb"])
                    for ch in range(8):
                        ls = slice(ch * 64, ch * 64 + 64)
                        k.mm(ps[5][0:64, ls], R(DdT[:, ls]), R(X_sb[:, ls]), True, True, reads=["DdT", "X_sb"], writes=["ps5"])
                        k.mm(ps[6][0:64, ls], R(Dd[:, ls]), R(Z_sb[:, ls]), True, True, reads=["Dd", "Z_sb"], writes=["ps6"])
                    k.tt("dve", R(Dd[:]), Dd[:], ps[5][0:64, :], ALU.subtract, reads=["Dd", "ps5"], writes=["Dd"])
                    k.tt("dve", R(DdT[:]), DdT[:], ps[6][0:64, :], ALU.subtract, reads=["DdT", "ps6"], writes=["DdT"])
                if stage < 5:
                    continue
                for ch in range(8):
                    ls = slice(ch * 64, ch * 64 + 64)
                    tc_ = tokc[:, 8 * ch:8 * ch + 8]
                    rt = [("tokc", ch)]
                    k.tr(ps[2][0:64, 0:128], k_fm[:, ls], ident, reads=["k_fm", "cst"], writes=["ps2"])
                    k.tr(ps[2][0:64, 128:256], v_fm[:, ls], ident, reads=["v_fm", "cst"], writes=["ps2"])
                    import os
                    SUB = int(os.environ.get("SUB", "9"))
                    if SUB < 1:
                        continue
                    k.ts("dve", R(rhs_w[:]), ps[2][0:64, 0:128], tc_[:, 4:5], None, ALU.mult, reads=["ps2"] + rt, writes=["rhs_w"])
                    if SUB < 2:
                        continue
                    k.ts("dve", R(kdec[:]), ps[2][0:64, 0:128], tc_[:, 3:4], None, ALU.mult, reads=["ps2"] + rt, writes=["kdec"])
                    k.ts("dve", R(rhs_u[:]), ps[2][0:64, 128:256], tc_[:, 1:2], None, ALU.mult, reads=["ps2"] + rt, writes=["rhs_u"])
                    if SUB < 3:
                        continue
                    k.tt("pool", R(qdec[:]), q_fm[:, ls], egcsb[:, ls], ALU.mult, reads=["q_fm", "egcsb"], writes=["qdec"])
                    if stage < 6:
                        continue
                    k.mm(ps[7][0:64, 0:128], R(DdT[:, ls]), R(rhs_u[:]), True, True, reads=["DdT", "rhs_u"], writes=["ps7"])
                    k.mm(ps[7][:, 128:192], R(rhs_w[:]), R(DdT[:, ls]), True, True, reads=["DdT", "rhs_w"], writes=["ps7"])
                    k.copy("act", u_sb[:], ps[7][0:64, 0:128], reads=["ps7"], writes=["u_sb"])
                    k.copy("dve", R(wT[:]), ps[7][:, 128:192], reads=["ps7"], writes=["wT"])
                    if stage < 7:
                        continue
                    k.mm(ps[7][0:64, 256:384], R(wT[:]), R(S_sb[:]), True, True, reads=["wT", "S"], writes=["ps7"])
                    k.tt("dve", R(vnew[:]), u_sb[:], ps[7][0:64, 256:384], ALU.subtract, reads=["u_sb", "ps7"], writes=["vnew"])
                    k.mm(ps[3][0:64, 0:128], R(qdec[:]), R(S_sb[:]), True, False, reads=["qdec", "S"], writes=["ps3"])
                    k.mm(ps[3][0:64, 0:128], R(qkT_all[:, ls]), R(vnew[:]), False, True, reads=["qkT_all", "vnew"], writes=["ps3"])
                    k.mm(ps[7][:, 384:512], R(kdec[:]), R(vnew[:]), True, True, reads=["kdec", "vnew"], writes=["ps7"])
                    k.stt("dve", R(S_sb[:]), S_sb[:], egcsb[:, ch * 64 + 63:ch * 64 + 64], ps[7][:, 384:512], ALU.mult, ALU.add,
                          reads=["S", "egcsb", "ps7"], writes=["S"])
                    if stage < 8:
                        continue
                    k.copy("act", o_sb[:], ps[3][0:64, 0:128], reads=["ps3"], writes=["o_sb"])
                    k.act(osq[:], o_sb[:], AF.Square, accum_out=tc_[:, 5:6], reads=["o_sb"], writes=["osq"] + rt)
                    k.act(tc_[:, 5:6], tc_[:, 5:6], AF.Sqrt, scale=1.0 / 128, bias=sm[0:64, 61:62],
                          reads=rt + ["eps6"], writes=rt)
                    k.P.op("dve", lambda e, a=tc_[:, 5:6]: e.reciprocal(a, a), reads=rt, writes=rt)
                    k.stt("dve", o_sb[:], o_sb[:], tc_[:, 5:6], nwb[:], ALU.mult, ALU.mult, reads=["o_sb", "nwb"] + rt, writes=["o_sb"])
                    k.tr(ps[3][:, 128:192], o_sb[:], ident[0:64, 0:64], reads=["o_sb", "cst"], writes=["ps3"])
                    k.tt("dve", yd_blk[:, ls], ps[3][:, 128:192], zs[:, ls], ALU.mult, reads=["ps3", "zs"], writes=["yd_blk"])
                k.dma("sp", yd_o[:, tok0:tok0 + 512], yd_blk[:], reads=["yd_blk"], writes=[("ydo", tok0)])
        P.final_waits("sp")
        P.emit()
    return nc


def cd_cst():
    cst = np.zeros((128, 1792), np.float32)
    cst[:, 0:128] = 1.0
    cst[:, 128:256] = np.eye(128, dtype=np.float32)
    cst[:, 256:768] = np.arange(512, dtype=np.float32)[None, :]
    s = np.arange(64)[:, None]
    l = np.arange(64)[None, :]
    cst[0:64, 768:832] = (s <= l)
    cst[0:64, 832:896] = (s < l)
    cst[0:64, 896:960] = (s > l)
    rm = np.ones(512, np.float32)
    rm[::64] = 0.0
    cst[0:1, 1024:1536] = rm[None, :]
    lv = np.zeros((64, 768), np.float32)
    i = np.arange(64)[:, None]
    j = np.arange(64)[None, :]
    for kk in range(6):
        bsz = 2 ** kk
        m = ((i // (2 * bsz)) == (j // (2 * bsz))) & ((i % (2 * bsz)) >= bsz) & ((j % (2 * bsz)) < bsz)
        lv[:, 64 * kk:64 * kk + 64] = m
        lv[:, 384 + 64 * kk:384 + 64 * kk + 64] = m.T
    return cst, lv


def p1cd_host(g, c):
    W = g("cd_w_in")[0]
    cols = np.concatenate([np.arange(c * 128, (c + 1) * 128), 1024 + np.arange(c * 128, (c + 1) * 128),
                           2048 + np.arange(c * 128, (c + 1) * 128), 3072 + np.arange(c * 128, (c + 1) * 128),
                           4096 + np.arange(c * 128, (c + 1) * 128), [5120 + c], [5128 + c]])
    w1 = np.ascontiguousarray(W[:, cols])
    pq = np.zeros((128, 64), np.float32)
    are, aim, ls_ = g("s5_a_re")[0], g("s5_a_im")[0], g("s5_log_step")[0]
    s5m = np.zeros((128, 2048), np.float32)
    bre, bim, cre, cim = g("s5_b_re")[0], g("s5_b_im")[0], g("s5_c_re")[0], g("s5_c_im")[0]
    for i in range(4):
        for gl in range(2):
            gg = 8 * c + 2 * i + gl
            pq[gl * 64:(gl + 1) * 64, i] = are[gg]
            pq[gl * 64:(gl + 1) * 64, 4 + i] = aim[gg]
            pq[gl * 64:(gl + 1) * 64, 8 + i] = ls_[gg]
            chs = slice(32 * i + 16 * gl, 32 * i + 16 * gl + 16)
            sts = slice(i * 128 + gl * 64, i * 128 + gl * 64 + 64)
            s5m[chs, sts] = bre[gg].T
            s5m[chs, 512 + i * 128 + gl * 64:512 + i * 128 + gl * 64 + 64] = bim[gg].T
            s5m[gl * 64:(gl + 1) * 64, 1024 + i * 128 + 32 * i + 16 * gl:1024 + i * 128 + 32 * i + 16 * gl + 16] = cre[gg].T
            s5m[gl * 64:(gl + 1) * 64, 1536 + i * 128 + 32 * i + 16 * gl:1536 + i * 128 + 32 * i + 16 * gl + 16] = cim[gg].T
    pq[:, 12] = g("s5_d")[0][c * 128:(c + 1) * 128]
    cw = g("dn_conv_w")[0]
    pq[:, 16:20] = cw[:, c * 128:(c + 1) * 128].T
    pq[:, 20:24] = cw[:, 1024 + c * 128:1024 + (c + 1) * 128].T
    pq[:, 24:28] = cw[:, 2048 + c * 128:2048 + (c + 1) * 128].T
    pq[0, 28] = g("dn_a_log")[0][c]
    pq[0, 29] = g("dn_dt_bias")[0][c]
    cst, lv = cd_cst()
    return {"w1": w1, "pq": pq, "cst": cst, "lv": lv, "s5m": s5m, "nw": g("dn_norm_w")[0][None, :].astype(np.float32)}


HD_SCALE = 512 ** -0.5
BIG = 1.0e9


def build_p2(kind, nblk=2, do_moe=True, do_xattn=True):
    ab = kind == "ab"
    KY = 3072 if ab else 2048
    KYC = KY // 128
    nc = bass.Bass("TRN2", target_bir_lowering=False)

    def D(name, shape, kind="ExternalInput"):
        return nc.dram_tensor(name, shape, F32, kind=kind).ap()

    xT = D("xT", [2048, 1024])
    yT = D("yT", [KY, 1024])
    memT = D("memT", [2048, 256])
    w_out = D("w_out", [KY, 2048])
    w_q = D("w_q", [2048, 2048])
    w_kv = D("w_kv", [2048, 4096])
    w_o = D("w_o", [2048, 2048])
    w_r = D("w_r", [2048, 36])
    b_r = D("b_r", [1, 36])
    w_gate = D("w_gate", [32, 2048, 256])
    w_up = D("w_up", [32, 2048, 256])
    w_down = D("w_down", [32, 256, 2048])
    pp_d = D("pp", [128, 112])
    cst_d = D("cst", [128, 256])
    if not ab:
        glu_w = D("glu_w", [1024, 1024])
    xo = D("xo", [2048, 1024], "ExternalOutput")

    with contextlib.ExitStack() as es:
        def S(name, shape):
            return es.enter_context(nc.sbuf_tensor(name + "_s", shape, F32))

        x_sb = S("x_sb", [128, 16 * 512])
        bufY = S("bufY", [128, 24 * 512])
        V_sb = S("V_sb", [128, 2 * 2048])
        ring = [S(f"ring{i}", [128, 4096]) for i in range(3)]
        q_sb = S("q_sb", [128, 4 * 512])
        e_sb = S("e_sb", [128, 4 * 256])
        pT_sb = S("pT_sb", [128, 2 * 512])
        sqt = [S(f"sq{i}", [128, 512]) for i in range(2)]
        t1t = [S(f"t1_{i}", [128, 512]) for i in range(2)]
        t2t = [S(f"t2_{i}", [128, 512]) for i in range(2)]
        h_sb = [S(f"h{i}", [128, 2 * 512]) for i in range(2)]
        cbc = [S(f"cbc{i}", [128, 512]) for i in range(2)]
        mean = S("mean", [128, 512])
        msq = S("msq", [128, 512])
        rstd = S("rstd", [128, 512])
        nmr = S("nmr", [128, 512])
        pp = S("pp_sb", [128, 112])
        cst = S("cst_sb", [128, 256])
        wr_sb = S("wr_sb", [128, 16 * 36])
        brb = S("brb", [128, 36])
        combT = S("combT", [32, 512])
        selb = [S(f"sel{i}", [32, 128]) for i in range(2)]
        sm = S("sm", [128, 256])
        ps = [es.enter_context(nc.psum_tensor(f"ps{i}", [128, 512], F32)) for i in range(8)]

        ones = cst[:, 0:128]
        ident = cst[:, 128:256]
        x3 = x_sb[:].rearrange("p (c t) -> p c t", t=512)
        y3 = bufY[:].rearrange("p (c t) -> p c t", t=512)
        kT3 = bufY[:, 16 * 512:24 * 512].rearrange("p (c m) -> p c m", m=256)
        mem3 = bufY[:, 0:4096].rearrange("p (c m) -> p c m", m=256)
        V3 = V_sb[:].rearrange("p (mc d) -> p mc d", d=2048)
        q3 = q_sb[:].rearrange("p (c t) -> p c t", t=512)
        e3 = e_sb[:].rearrange("p (tt m) -> p tt m", m=256)
        pT3 = pT_sb[:].rearrange("p (mc t) -> p mc t", t=512)
        wr3 = wr_sb[:].rearrange("p (c n) -> p c n", n=36)

        import os
        P = Prog(nc, same_engine_sync=os.environ.get("SAME", "1") == "1")
        k = K(P)
        state = {"slot": 0}

        def wslot():
            i = state["slot"] % 3
            state["slot"] += 1
            return ring[i], f"ring{i}"

        def wload(src_ap, kc, ncol):
            t, rn = wslot()
            v = t[:, 0:kc * ncol].rearrange("p (c j) -> p c j", j=ncol)
            k.dma("qpool", R(v), src_ap.rearrange("(c p) j -> p c j", p=128), writes=[rn])
            return v, rn

        epsb = S("epsb", [128, 2])
        k.memset("dve", epsb[:, 0:1], LN_EPS, writes=["epsb"])
        k.memset("dve", epsb[:, 1:2], RMS_EPS, writes=["epsb"])
        k.dma("sp", pp[:], pp_d[:, :], writes=["pp"])
        k.dma("qpool", R(cst[:]), cst_d[:, :], writes=["cst"])
        k.dma("sp", wr3, w_r.rearrange("(c p) n -> p c n", p=128), writes=["wr"])
        k.dma("sp", brb[:], b_r[0].partition_broadcast(128), writes=["brb"])

        def layer_norm(g0, b0):
            for j in range(16):
                k.mm(ps[3][:], R(ones), R(x3[:, j, :]), j == 0, j == 15,
                     reads=[("x", j), "cst"], writes=["ps3"])
            for j in range(16):
                sq = sqt[j % 2]
                k.act(R(sq[:]), x3[:, j, :], AF.Square, reads=[("x", j)], writes=[("sq", j % 2)])
                k.mm(ps[4][:], R(ones), R(sq[:]), j == 0, j == 15,
                     reads=[("sq", j % 2), "cst"], writes=["ps4"])
            k.act(mean[:], ps[3][:], AF.Copy, scale=1.0 / 2048, reads=["ps3"], writes=["mean"])
            k.act(msq[:], ps[3][:], AF.Square, scale=1.0 / 2048, reads=["ps3"], writes=["msq"])
            k.stt("dve", rstd[:], ps[4][:], 1.0 / 2048, msq[:], ALU.mult, ALU.subtract,
                  reads=["ps4", "msq"], writes=["rstd"])
            k.act(rstd[:], rstd[:], AF.Sqrt, bias=epsb[:, 0:1], reads=["rstd", "epsb"], writes=["rstd"])
            k.P.op("dve", lambda e: e.reciprocal(rstd[:], rstd[:]), reads=["rstd"], writes=["rstd"])
            k.stt("dve", nmr[:], mean[:], -1.0, rstd[:], ALU.mult, ALU.mult,
                  reads=["mean", "rstd"], writes=["nmr"])
            for j in range(16):
                t1 = t1t[j % 2]
                t2 = t2t[j % 2]
                k.tt("dve", t1[:], x3[:, j, :], rstd[:], ALU.mult,
                     reads=[("x", j), "rstd"], writes=[("t1", j % 2)])
                k.tt("pool", t2[:], t1[:], nmr[:], ALU.add,
                     reads=[("t1", j % 2), "nmr"], writes=[("t2", j % 2)])
                k.act(R(x3[:, j, :]), t2[:], AF.Identity, scale=pp[:, g0 + j:g0 + j + 1],
                      bias=pp[:, b0 + j:b0 + j + 1], reads=[("t2", j % 2), "pp"], writes=[("x", j)])

        def proj_residual(w_dram, kyc, rhs_of, rhs_res_of):
            ncol = 128 if kyc == 24 else 256
            per = ncol // 128
            for jt in range(2048 // ncol):
                wv, rn = wload(w_dram[:, jt * ncol:(jt + 1) * ncol], kyc, ncol)
                for jj in range(per):
                    j = jt * per + jj
                    pn = 1 + j % 2
                    for kc in range(kyc):
                        k.mm(ps[pn][:], R(wv[:, kc, jj * 128:(jj + 1) * 128]), R(rhs_of(kc)),
                             kc == 0, kc == kyc - 1, reads=[rn, rhs_res_of(kc)], writes=[f"ps{pn}"])
                    k.stt("dve", R(x3[:, j, :]), x3[:, j, :], ALPHA, ps[pn][:], ALU.mult, ALU.add,
                          reads=[("x", j), f"ps{pn}"], writes=[("x", j)])

        for blk in range(nblk):
            t0 = blk * 512
            for q4 in range(4):
                k.dma("qpool", R(x3[:, 4 * q4:4 * q4 + 4, :]),
                      xT[q4 * 512:(q4 + 1) * 512, t0:t0 + 512].rearrange("(c p) t -> p c t", p=128),
                      writes=[("x", 4 * q4 + i) for i in range(4)])
            for q4 in range(KYC // 4):
                k.dma("qpool", R(y3[:, 4 * q4:4 * q4 + 4, :]),
                      yT[q4 * 512:(q4 + 1) * 512, t0:t0 + 512].rearrange("(c p) t -> p c t", p=128),
                      writes=[("y", 4 * q4 + i) for i in range(4)])
            if ab:
                for g in range(2):
                    for i in range(8):
                        j = 8 + 8 * g + i
                        sq = sqt[i % 2]
                        k.act(R(sq[:]), y3[:, j, :], AF.Square, reads=[("y", j)], writes=[("sq", i % 2)])
                        k.mm(ps[0][:], R(ones), R(sq[:]), i == 0, i == 7,
                             reads=[("sq", i % 2), "cst"], writes=["ps0"])
                    k.act(rstd[:], ps[0][:], AF.Sqrt, scale=1.0 / 1024, bias=epsb[:, 1:2],
                          reads=["ps0", "epsb"], writes=["rstd"])
                    k.P.op("dve", lambda e: e.reciprocal(rstd[:], rstd[:]), reads=["rstd"], writes=["rstd"])
                    for i in range(8):
                        j = 8 + 8 * g + i
                        k.stt("dve", R(y3[:, j, :]), y3[:, j, :], pp[:, 96 + j - 8:97 + j - 8], rstd[:],
                              ALU.mult, ALU.mult, reads=[("y", j), "rstd", "pp"], writes=[("y", j)])
                rhs_of = lambda kc: y3[:, kc, :]
                rhs_res_of = lambda kc: ("y", kc)
            else:
                for j2 in range(4):
                    wv, rn = wload(glu_w[:, j2 * 256:(j2 + 1) * 256], 8, 256)
                    for jj in range(2):
                        j = 2 * j2 + jj
                        pn = 5 + jj
                        for kc in range(8):
                            k.mm(ps[pn][:], R(wv[:, kc, jj * 128:(jj + 1) * 128]), R(y3[:, kc, :]),
                                 kc == 0, kc == 7, reads=[rn, ("y", kc)], writes=[f"ps{pn}"])
                        t1 = t1t[j % 2]
                        k.act(t1[:], ps[pn][:], AF.Sigmoid, bias=pp[:, 96 + j:97 + j],
                              reads=[f"ps{pn}", "pp"], writes=[("t1", j % 2)])
                        k.tt("dve", R(y3[:, 16 + j, :]), t1[:], y3[:, j, :], ALU.mult,
                             reads=[("t1", j % 2), ("y", j)], writes=[("y", 16 + j)])
                rhs_of = lambda kc: y3[:, 16 + kc, :] if kc < 8 else y3[:, kc, :]
                rhs_res_of = lambda kc: ("y", 16 + kc) if kc < 8 else ("y", kc)
            proj_residual(w_out, KYC, rhs_of, rhs_res_of)
            layer_norm(0, 16)

            if do_xattn:
                for q4 in range(4):
                    k.dma("qpool", R(mem3[:, 4 * q4:4 * q4 + 4, :]),
                          memT[q4 * 512:(q4 + 1) * 512, :].rearrange("(c p) m -> p c m", p=128),
                          writes=[("y", 2 * q4), ("y", 2 * q4 + 1)])
                memres = [("y", i) for i in range(8)]
                for kt in range(8):
                    wv, rn = wload(w_kv[:, kt * 256:(kt + 1) * 256], 16, 256)
                    for jj in range(2):
                        c = 2 * kt + jj
                        pn = c % 4
                        for kc in range(16):
                            k.mm(ps[pn][:, 0:256], R(wv[:, kc, jj * 128:(jj + 1) * 128]), R(mem3[:, kc, :]),
                                 kc == 0, kc == 15, reads=[rn, ("y", kc // 2)], writes=[f"ps{pn}"])
                        k.copy("act", R(kT3[:, c, :]), ps[pn][:, 0:256],
                               reads=[f"ps{pn}"], writes=[("y", 16 + c // 2)])
                if blk == 0:
                    for vt in range(8):
                        wv, rn = wload(w_kv[:, 2048 + vt * 256:2048 + (vt + 1) * 256], 16, 256)
                        for mc in range(2):
                            pn = (2 * vt + mc) % 4
                            for kc in range(16):
                                k.mm(ps[pn][:, 0:256], R(mem3[:, kc, mc * 128:(mc + 1) * 128]), R(wv[:, kc, :]),
                                     kc == 0, kc == 15, reads=[rn, ("y", kc // 2)], writes=[f"ps{pn}"])
                            k.copy("dve", R(V3[:, mc, vt * 256:(vt + 1) * 256]), ps[pn][:, 0:256],
                                   reads=[f"ps{pn}"], writes=["V"])
                for h in range(4):
                    for qt in range(2):
                        wv, rn = wload(w_q[:, h * 512 + qt * 256:h * 512 + (qt + 1) * 256], 16, 256)
                        for jj in range(2):
                            c = 2 * qt + jj
                            pn = c % 4
                            for kc in range(16):
                                k.mm(ps[pn][:], R(wv[:, kc, jj * 128:(jj + 1) * 128]), R(x3[:, kc, :]),
                                     kc == 0, kc == 15, reads=[rn, ("x", kc)], writes=[f"ps{pn}"])
                            k.act(R(q3[:, c, :]), ps[pn][:], AF.Copy, scale=HD_SCALE,
                                  reads=[f"ps{pn}"], writes=[("q", c)])
                    for tt in range(4):
                        pn = 4 + tt // 2
                        for c in range(4):
                            k.mm(ps[pn][:, (tt % 2) * 256:(tt % 2 + 1) * 256],
                                 R(q3[:, c, tt * 128:(tt + 1) * 128]), R(kT3[:, h * 4 + c, :]),
                                 c == 0, c == 3, reads=[("q", c), ("y", 16 + (h * 4 + c) // 2)],
                                 writes=[f"ps{pn}"])
                    mx = sm[:, 0:4]
                    nmx = sm[:, 4:8]
                    ssum = sm[:, 8:12]
                    rs = sm[:, 12:16]
                    for half in range(2):
                        k.reduce("dve", mx[:, 2 * half:2 * half + 2],
                                 ps[4 + half][:].rearrange("p (a m) -> p a m", m=256), ALU.max,
                                 reads=[f"ps{4 + half}"], writes=["mx"])
                    k.ts("dve", nmx, mx, -1.0, None, ALU.mult, reads=["mx"], writes=["nmx"])
                    for tt in range(4):
                        pn = 4 + tt // 2
                        k.act(e3[:, tt, :], ps[pn][:, (tt % 2) * 256:(tt % 2 + 1) * 256], AF.Exp,
                              bias=nmx[:, tt:tt + 1], accum_out=ssum[:, tt:tt + 1],
                              reads=[f"ps{pn}", "nmx"], writes=[("e", tt), "ssum"])
                    k.P.op("dve", lambda e, rs=rs, ssum=ssum: e.reciprocal(rs, ssum), reads=["ssum"], writes=["rs"])
                    for tt in range(4):
                        k.ts("dve" if tt % 2 == 0 else "pool", e3[:, tt, :], e3[:, tt, :], rs[:, tt:tt + 1], None,
                             ALU.mult, reads=[("e", tt), "rs"], writes=[("e", tt)])
                    for mc in range(2):
                        for tt in range(4):
                            k.tr(ps[6 + mc][:, tt * 128:(tt + 1) * 128], e3[:, tt, mc * 128:(mc + 1) * 128], ident,
                                 reads=[("e", tt), "cst"], writes=[f"ps{6 + mc}"])
                        k.copy("act" if mc == 0 else "dve", R(pT3[:, mc, :]), ps[6 + mc][:],
                               reads=[f"ps{6 + mc}"], writes=[("pT", mc)])
                    for c in range(4):
                        pn = c % 4
                        for mc in range(2):
                            k.mm(ps[pn][:], R(V3[:, mc, (h * 4 + c) * 128:(h * 4 + c + 1) * 128]), R(pT3[:, mc, :]),
                                 mc == 0, mc == 1, reads=["V", ("pT", mc)], writes=[f"ps{pn}"])
                        k.copy("act", R(y3[:, h * 4 + c, :]), ps[pn][:],
                               reads=[f"ps{pn}"], writes=[("y", h * 4 + c)])
                proj_residual(w_o, 16, lambda kc: y3[:, kc, :], lambda kc: ("y", kc))
                layer_norm(32, 48)

            if do_moe:
                for tt in range(4):
                    for kc in range(16):
                        k.mm(ps[0][:, tt * 36:(tt + 1) * 36], x3[:, kc, tt * 128:(tt + 1) * 128], wr3[:, kc, :],
                             kc == 0, kc == 15, reads=[("x", kc), "wr"], writes=["ps0"])
                for tt in range(4):
                    lg = sm[:, 16:52]
                    gl = sm[:, 16:20]
                    el = sm[:, 20:52]
                    gmax = sm[:, 52:53]
                    ngmax = sm[:, 53:54]
                    gsum = sm[:, 54:55]
                    gp = sm[:, 55:56]
                    m1 = sm[:, 56:57]
                    m2 = sm[:, 57:58]
                    d21 = sm[:, 58:59]
                    w1 = sm[:, 59:60]
                    w2 = sm[:, 60:61]
                    ohg = sm[:, 64:68]
                    pen = sm[:, 68:72]
                    tmp4 = sm[:, 72:76]
                    elm = sm[:, 80:112]
                    oh1 = sm[:, 112:144]
                    elm2 = sm[:, 144:176]
                    oh2 = sm[:, 176:208]
                    comb = sm[:, 208:240]
                    rr = ["smr"]
                    k.tt("dve", lg, ps[0][:, tt * 36:(tt + 1) * 36], brb[:], ALU.add,
                         reads=["ps0", "brb"], writes=rr)
                    k.reduce("dve", gmax, gl, ALU.max, reads=rr, writes=rr)
                    k.ts("dve", ohg, gl, gmax, None, ALU.is_equal, reads=rr, writes=rr)
                    k.ts("dve", ngmax, gmax, -1.0, None, ALU.mult, reads=rr, writes=rr)
                    k.act(tmp4, gl, AF.Exp, bias=ngmax, accum_out=gsum, reads=rr, writes=rr)
                    k.P.op("dve", lambda e, gp=gp, gsum=gsum: e.reciprocal(gp, gsum), reads=rr, writes=rr)
                    k.ts("dve", pen, ohg, BIG, -BIG, ALU.mult, ALU.add, reads=rr, writes=rr)
                    k.tt("dve", elm.rearrange("p (g e) -> p g e", e=8), el.rearrange("p (g e) -> p g e", e=8),
                         pen.unsqueeze(2).to_broadcast([128, 4, 8]), ALU.add, reads=rr, writes=rr)
                    k.reduce("dve", m1, elm, ALU.max, reads=rr, writes=rr)
                    k.ts("dve", oh1, elm, m1, None, ALU.is_equal, reads=rr, writes=rr)
                    k.stt("dve", elm2, oh1, -BIG, elm, ALU.mult, ALU.add, reads=rr, writes=rr)
                    k.reduce("dve", m2, elm2, ALU.max, reads=rr, writes=rr)
                    k.ts("dve", oh2, elm2, m2, None, ALU.is_equal, reads=rr, writes=rr)
                    k.tt("dve", d21, m2, m1, ALU.subtract, reads=rr, writes=rr)
                    k.act(w1, d21, AF.Exp, reads=rr, writes=rr)
                    k.ts("dve", w1, w1, 1.0, None, ALU.add, reads=rr, writes=rr)
                    k.P.op("dve", lambda e, w1=w1: e.reciprocal(w1, w1), reads=rr, writes=rr)
                    k.tt("dve", w1, w1, gp, ALU.mult, reads=rr, writes=rr)
                    k.tt("dve", w2, gp, w1, ALU.subtract, reads=rr, writes=rr)
                    k.ts("dve", comb, oh1, w1, None, ALU.mult, reads=rr, writes=rr)
                    k.stt("dve", comb, oh2, w2, comb, ALU.mult, ALU.add, reads=rr, writes=rr)
                    k.tr(ps[1][0:32, tt * 128:(tt + 1) * 128], comb, ident, reads=rr + ["cst"], writes=["ps1"])
                k.copy("act", R(combT[:]), ps[1][0:32, :], reads=["ps1"], writes=["combT"])
                for e in range(32):
                    sb = selb[e % 2]
                    cb = cbc[e % 2]
                    hh = h_sb[e % 2]
                    h3 = hh[:].rearrange("p (f t) -> p f t", t=512)
                    k.copy("dve", R(sb[:]), ident[0:32, e:e + 1].to_broadcast([32, 128]),
                           reads=["cst"], writes=[("sel", e % 2)])
                    k.mm(ps[2][:], R(sb[:]), R(combT[:]), True, True,
                         reads=[("sel", e % 2), "combT"], writes=["ps2"])
                    k.copy("act", cb[:], ps[2][:], reads=["ps2"], writes=[("cbc", e % 2)])
                    wg, rg = wload(w_gate[e], 16, 256)
                    wu, ru = wload(w_up[e], 16, 256)
                    td, rd = wslot()
                    wd = td[:].rearrange("p (f d) -> p f d", d=2048)
                    k.dma("qpool", R(wd), w_down[e].rearrange("(f p) d -> p f d", p=128), writes=[rd])
                    for fc in range(2):
                        gpn = 3 + 2 * fc
                        upn = 4 + 2 * fc
                        for kc in range(16):
                            k.mm(ps[gpn][:], R(wg[:, kc, fc * 128:(fc + 1) * 128]), R(x3[:, kc, :]),
                                 kc == 0, kc == 15, reads=[rg, ("x", kc)], writes=[f"ps{gpn}"])
                        for kc in range(16):
                            k.mm(ps[upn][:], R(wu[:, kc, fc * 128:(fc + 1) * 128]), R(x3[:, kc, :]),
                                 kc == 0, kc == 15, reads=[ru, ("x", kc)], writes=[f"ps{upn}"])
                        t1 = t1t[fc]
                        t2 = t2t[fc]
                        k.act(t1[:], ps[gpn][:], AF.Silu, reads=[f"ps{gpn}"], writes=[("t1", fc)])
                        k.tt("dve", t2[:], t1[:], ps[upn][:], ALU.mult,
                             reads=[("t1", fc), f"ps{upn}"], writes=[("t2", fc)])
                        k.tt("pool", R(h3[:, fc, :]), t2[:], cb[:], ALU.mult,
                             reads=[("t2", fc), ("cbc", e % 2)], writes=[("h", e % 2, fc)])
                    for j in range(16):
                        dn = [7, 0, 1][j % 3]
                        for fc in range(2):
                            k.mm(ps[dn][:], R(wd[:, fc, j * 128:(j + 1) * 128]), R(h3[:, fc, :]),
                                 fc == 0, fc == 1, reads=[rd, ("h", e % 2, fc)], writes=[f"ps{dn}"])
                        if e == 0:
                            k.copy("dve", R(y3[:, j, :]), ps[dn][:], reads=[f"ps{dn}"], writes=[("y", j)])
                        else:
                            k.tt("dve", R(y3[:, j, :]), ps[dn][:], y3[:, j, :], ALU.add,
                                 reads=[f"ps{dn}", ("y", j)], writes=[("y", j)])
                for j in range(16):
                    k.stt("dve", R(x3[:, j, :]), x3[:, j, :], ALPHA, y3[:, j, :], ALU.mult, ALU.add,
                          reads=[("x", j), ("y", j)], writes=[("x", j)])
                layer_norm(64, 80)

            for q4 in range(4):
                k.dma("sp", xo[q4 * 512:(q4 + 1) * 512, t0:t0 + 512].rearrange("(c p) t -> p c t", p=128),
                      x3[:, 4 * q4:4 * q4 + 4, :],
                      reads=[("x", 4 * q4 + i) for i in range(4)], writes=[("xo", blk, q4)])
        P.final_waits("sp")
        P.emit()
    return nc

from concourse.bass_utils import run_bass_kernel_spmd

_CACHE = {}


def _chunkpack(v):
    return np.ascontiguousarray(np.asarray(v, np.float32).reshape(-1, 128).T)


def _p2_inputs(kind, L, I, xT_c, yT_c, memT):
    g = lambda n: np.asarray(I[n])
    pp = np.zeros((128, 112), np.float32)
    for i, n in enumerate(["ln1_g", "ln1_b", "ln2_g", "ln2_b", "ln3_g", "ln3_b"]):
        pp[:, 16 * i:16 * i + 16] = _chunkpack(g(n)[L])
    if kind == "ab":
        pp[:, 96:112] = _chunkpack(g("ssd_norm_w")[0])
    else:
        pp[:, 96:104] = _chunkpack(g("s5_glu_b")[0])
    cst = np.concatenate([np.ones((128, 128), np.float32), np.eye(128, dtype=np.float32)], 1)
    d = {
        "xT": xT_c, "yT": yT_c, "memT": memT,
        "w_out": g("ab_w_out")[0] if kind == "ab" else g("cd_w_out")[0],
        "w_q": g("xa_w_q")[L], "w_kv": g("xa_w_kv")[L], "w_o": g("xa_w_o")[L],
        "w_r": np.ascontiguousarray(np.concatenate([g("moe_w_group")[L], g("moe_w_expert")[L]], 1)),
        "b_r": np.ascontiguousarray(np.concatenate([g("moe_b_group")[L], g("moe_b_expert")[L]])[None, :]),
        "w_gate": g("moe_w_gate")[L], "w_up": g("moe_w_up")[L], "w_down": g("moe_w_down")[L],
        "pp": pp, "cst": cst,
    }
    if kind == "cd":
        d["glu_w"] = g("s5_glu_w")[0]
    return d


def _get(name, fn):
    if name not in _CACHE:
        _CACHE[name] = fn()
    return _CACHE[name]


def _run_p2(kind, L, I, xT, yT, mem):
    nc = _get("p2" + kind, lambda: build_p2(kind))
    maps = []
    memTs = [np.ascontiguousarray(np.asarray(mem[b], np.float32).T) for b in range(2)]
    for c in range(8):
        sl = slice(c * 1024, (c + 1) * 1024)
        maps.append(_p2_inputs(kind, L, I, np.ascontiguousarray(xT[:, sl]), np.ascontiguousarray(yT[:, sl]),
                               memTs[c // 4]))
    res = run_bass_kernel_spmd(nc, maps, core_ids=list(range(8)))
    return np.concatenate([r["xo"] for r in res.results], axis=1)


def kernel(**I):
    g = lambda n: np.asarray(I[n], np.float32)
    x = g("x")
    mem = g("mem")
    xT = np.ascontiguousarray(x.reshape(8192, 2048).T)
    nc = _get("p1ab", build_p1ab)
    maps = []
    for c in range(8):
        d = p1ab_host(g, c)
        d["xT"] = xT
        maps.append(d)
    res = run_bass_kernel_spmd(nc, maps, core_ids=list(range(8)))
    yT = np.empty((3072, 8192), np.float32)
    for c in range(8):
        yT[c * 128:(c + 1) * 128] = res.results[c]["ya"]
        yT[1024 + c * 256:1024 + (c + 1) * 256] = res.results[c]["yb"].reshape(256, 8192)
    x1T = _run_p2("ab", 0, I, xT, yT, mem)
    nc = _get("p1cd", build_p1cd)
    maps = []
    for c in range(8):
        d = p1cd_host(g, c)
        d["xT"] = x1T
        maps.append(d)
    res = run_bass_kernel_spmd(nc, maps, core_ids=list(range(8)))
    yT = np.empty((2048, 8192), np.float32)
    for c in range(8):
        yT[c * 128:(c + 1) * 128] = res.results[c]["gs"]
        yT[1024 + c * 128:1024 + (c + 1) * 128] = res.results[c]["yd"]
    x2T = _run_p2("cd", 1, I, x1T, yT, mem)
    return np.ascontiguousarray(x2T.T).reshape(2, 4096, 2048).astype(np.float32)
```
